# Optimizing a Trainium2 kernel written in Bass

```python
import jax, jax.numpy as jnp
from jax import lax
import numpy as np

D_MODEL = 2048
BATCH = 4
SEQ = 2048
DEPTH = 1

CONV_DIM = 1024
CONV_GROUPS = 16
CONV_WIDTH = 3
N_HEADS = 8
HEAD_DIM = 128
ATTN_DIM = N_HEADS * HEAD_DIM
MIX_DIM = CONV_DIM + ATTN_DIM
KV_RANK = 512
IDX_HEADS = 16
IDX_DIM = 128
TOPK_MAX = 256
Q_BLOCK = 128
N_GROUPS = 4
EXPERTS_PER_GROUP = 8
N_EXPERTS = N_GROUPS * EXPERTS_PER_GROUP
TOPK_EXPERTS = 2
EXPERT_FF = 512
N_MOD = 6
EPS = 1e-6

OFF_B = 0
OFF_C = OFF_B + CONV_DIM
OFF_X = OFF_C + CONV_DIM
OFF_Q = OFF_X + CONV_DIM
OFF_KV = OFF_Q + ATTN_DIM
OFF_IQ = OFF_KV + KV_RANK
OFF_IK = OFF_IQ + IDX_HEADS * IDX_DIM
OFF_IW = OFF_IK + IDX_DIM
IN_COLS = OFF_IW + IDX_HEADS

kernel_name = "hymba_conv_dsa_hmoe_adaln"


def rmsnorm(x, g):
    xf = x.astype(jnp.float32)
    y = xf * lax.rsqrt(jnp.mean(xf * xf, axis=-1, keepdims=True) + EPS)
    return (y * g.astype(jnp.float32)).astype(x.dtype)


def causal_dwconv(u, w):
    return lax.conv_general_dilated(
        u, w[:, None, :], window_strides=(1,), padding=[(CONV_WIDTH - 1, 0)],
        dimension_numbers=('NWC', 'WIO', 'NWC'), feature_group_count=u.shape[-1])


def dsa_attention(q_lat, iq, iw, ik, ckv):
    B, S = q_lat.shape[0], q_lat.shape[1]
    k_sel = min(TOPK_MAX, S // 4)
    nb = S // Q_BLOCK
    scale = HEAD_DIM ** -0.5
    key_pos = jnp.arange(S, dtype=jnp.int32)
    q_pos = key_pos.reshape(nb, Q_BLOCK)

    def to_blocks(a):
        return jnp.moveaxis(a.reshape((B, nb, Q_BLOCK) + a.shape[2:]), 1, 0)

    def block(args):
        ql, qi, wi, qp = args
        s_idx = jnp.einsum('bqhd,bsd->bqhs', qi, ik, preferred_element_type=jnp.float32)
        idx_score = jnp.einsum('bqhs,bqh->bqs', jax.nn.relu(s_idx), wi.astype(jnp.float32))
        causal = key_pos[None, :] <= qp[:, None]
        idx_score = jnp.where(causal[None], idx_score, -jnp.inf)
        _, sel = lax.top_k(idx_score, k_sel)
        valid = sel <= qp[None, :, None]
        kv = jax.vmap(lambda cb, ib: cb[ib])(ckv, sel)
        logits = jnp.einsum('bqhr,bqkr->bqhk', ql, kv, preferred_element_type=jnp.float32) * scale
        logits = jnp.where(valid[:, :, None, :], logits, -jnp.inf)
        p = jax.nn.softmax(logits, axis=-1).astype(kv.dtype)
        return jnp.einsum('bqhk,bqkr->bqhr', p, kv)

    o = lax.map(block, (to_blocks(q_lat), to_blocks(iq), to_blocks(iw), q_pos))
    return jnp.moveaxis(o, 0, 1).reshape(B, S, N_HEADS, KV_RANK)


def hier_moe(h, w_rg, b_rg, w_re, b_re, w_gate, w_up, w_down):
    B, S, D = h.shape
    t = h.reshape(-1, D)
    g_prob = jax.nn.softmax((t @ w_rg).astype(jnp.float32) + b_rg.astype(jnp.float32), axis=-1)
    p_group, g_sel = lax.top_k(g_prob, 1)
    e_logits = ((t @ w_re).astype(jnp.float32) + b_re.astype(jnp.float32)).reshape(-1, N_GROUPS, EXPERTS_PER_GROUP)
    e_logits = jnp.take_along_axis(e_logits, g_sel[:, :, None], axis=1)[:, 0]
    top_p, top_i = lax.top_k(jax.nn.softmax(e_logits, axis=-1), TOPK_EXPERTS)
    top_p = top_p / jnp.sum(top_p, axis=-1, keepdims=True)
    within = jnp.einsum('tk,tke->te', top_p, jax.nn.one_hot(top_i, EXPERTS_PER_GROUP, dtype=jnp.float32))
    combine = (jax.nn.one_hot(g_sel[:, 0], N_GROUPS, dtype=jnp.float32)[:, :, None]
               * (p_group * within)[:, None, :]).reshape(-1, N_EXPERTS).astype(t.dtype)
    out = jnp.zeros_like(t)
    for g in range(N_GROUPS):
        sl = slice(g * EXPERTS_PER_GROUP, (g + 1) * EXPERTS_PER_GROUP)
        a = jnp.einsum('td,edf->tef', t, w_gate[sl])
        u = jnp.einsum('td,edf->tef', t, w_up[sl])
        hid = jax.nn.silu(a) * u * combine[:, sl, None]
        out = out + jnp.einsum('tef,efd->td', hid, w_down[sl])
    return out.reshape(B, S, D)


def setup_inputs(seed: int = 0) -> dict:
    key = jax.random.key(seed)
    ks = jax.random.split(key, 24)
    f = jnp.float32
    L, D = DEPTH, D_MODEL

    def nrm(k, shape, std):
        return jax.random.normal(k, shape, f) * std

    def gain(k, shape):
        return 1.0 + 0.02 * jax.random.normal(k, shape, f)

    return {
        "x": nrm(ks[0], (BATCH, SEQ, D), 1.0),
        "c": nrm(ks[1], (BATCH, D), 1.0),
        "w_ada": nrm(ks[2], (L, D, N_MOD * D), 0.5 * D ** -0.5),
        "b_ada": nrm(ks[3], (L, N_MOD * D), 0.02),
        "g_mix": gain(ks[4], (L, D)),
        "w_in": nrm(ks[5], (L, D, IN_COLS), D ** -0.5),
        "conv_w": nrm(ks[6], (L, CONV_WIDTH, CONV_DIM), CONV_WIDTH ** -0.5),
        "w_uk": nrm(ks[7], (L, N_HEADS, KV_RANK, HEAD_DIM), KV_RANK ** -0.5),
        "kv_norm_g": gain(ks[8], (L, KV_RANK)),
        "w_uv": nrm(ks[9], (L, N_HEADS, KV_RANK, HEAD_DIM), KV_RANK ** -0.5),
        "g_conv_out": gain(ks[10], (L, CONV_DIM)),
        "g_attn_out": gain(ks[11], (L, ATTN_DIM)),
        "w_out": nrm(ks[12], (L, MIX_DIM, D), MIX_DIM ** -0.5),
        "g_ffn": gain(ks[13], (L, D)),
        "w_rg": nrm(ks[14], (L, D, N_GROUPS), D ** -0.5),
        "b_rg": nrm(ks[15], (L, N_GROUPS), 0.01),
        "w_re": nrm(ks[16], (L, D, N_EXPERTS), D ** -0.5),
        "b_re": nrm(ks[17], (L, N_EXPERTS), 0.01),
        "w_gate": nrm(ks[18], (L, N_EXPERTS, D, EXPERT_FF), D ** -0.5),
        "w_up": nrm(ks[19], (L, N_EXPERTS, D, EXPERT_FF), D ** -0.5),
        "w_down": nrm(ks[20], (L, N_EXPERTS, EXPERT_FF, D), EXPERT_FF ** -0.5),
        "w_ada_f": nrm(ks[21], (D, 2 * D), 0.5 * D ** -0.5),
        "b_ada_f": nrm(ks[22], (2 * D,), 0.02),
        "g_final": gain(ks[23], (D,)),
    }


def reference(x, c, w_ada, b_ada, g_mix, w_in, conv_w, w_uk, kv_norm_g, w_uv, g_conv_out, g_attn_out,
              w_out, g_ffn, w_rg, b_rg, w_re, b_re, w_gate, w_up, w_down, w_ada_f, b_ada_f, g_final):
    B, S, D = x.shape
    c_act = jax.nn.silu(c)
    for l in range(DEPTH):
        mod = (c_act @ w_ada[l] + b_ada[l])[:, None, :]
        sh1, sc1, gt1, sh2, sc2, gt2 = jnp.split(mod, N_MOD, axis=-1)

        h = rmsnorm(x, g_mix[l]) * (1.0 + sc1) + sh1
        proj = h @ w_in[l]

        bg = proj[..., OFF_B:OFF_C]
        cg = proj[..., OFF_C:OFF_X]
        xv = proj[..., OFF_X:OFF_Q]
        y_conv = bg * causal_dwconv(cg * xv, conv_w[l])

        q = proj[..., OFF_Q:OFF_KV].reshape(B, S, N_HEADS, HEAD_DIM)
        ckv = rmsnorm(proj[..., OFF_KV:OFF_IQ], kv_norm_g[l])
        iq = proj[..., OFF_IQ:OFF_IK].reshape(B, S, IDX_HEADS, IDX_DIM)
        ik = proj[..., OFF_IK:OFF_IW]
        iw = proj[..., OFF_IW:IN_COLS] * (IDX_HEADS ** -0.5 * IDX_DIM ** -0.5)
        q_lat = jnp.einsum('bshd,hrd->bshr', q, w_uk[l])
        o_lat = dsa_attention(q_lat, iq, iw, ik, ckv)
        y_attn = jnp.einsum('bshr,hrd->bshd', o_lat, w_uv[l]).reshape(B, S, ATTN_DIM)

        mix = jnp.concatenate([rmsnorm(y_conv, g_conv_out[l]), rmsnorm(y_attn, g_attn_out[l])], axis=-1)
        x = x + gt1 * (mix @ w_out[l])

        h2 = rmsnorm(x, g_ffn[l]) * (1.0 + sc2) + sh2
        x = x + gt2 * hier_moe(h2, w_rg[l], b_rg[l], w_re[l], b_re[l], w_gate[l], w_up[l], w_down[l])

    modf = (c_act @ w_ada_f + b_ada_f)[:, None, :]
    shf, scf = jnp.split(modf, 2, axis=-1)
    return rmsnorm(x, g_final) * (1.0 + scf) + shf
```

```python
import numpy as np
import ml_dtypes
from contextlib import ExitStack
import concourse.bass as bass
import concourse.mybir as mybir
from concourse.bass_utils import run_bass_kernel_spmd

F32 = mybir.dt.float32
BF16 = mybir.dt.bfloat16
AF = mybir.ActivationFunctionType
ALU = mybir.AluOpType
AX = mybir.AxisListType

D = 2048
SEQ = 2048
NT = 1024
KC = 16
IN_COLS = 6800
OFF_KV, OFF_IQ, OFF_IK, OFF_IW = 4096, 4608, 6656, 6784
NEXP = 32
FF = 512
TOPK = 256
NIT = 24
MG = 512
BIG = 1.0e30
EPS = 1e-6
ATT_SCALE = 128 ** -0.5
IW_SCALE = (16 ** -0.5) * (128 ** -0.5)

ENGS = ("pe", "act", "dve", "pool", "sp")
SEM_WINDOW = 3000
N_DMA_SEMS = 34
N_SWDMA_SEMS = 10


class Buf:
    __slots__ = ("name", "writer", "readers", "dsem", "dcount", "persist", "dkind")

    def __init__(self, name, persist=False):
        self.name = name
        self.writer = None
        self.readers = []
        self.dsem = None
        self.dcount = 0
        self.persist = persist
        self.dkind = None


class Op:
    __slots__ = ("eng", "fn", "deps", "is_dma", "ndma", "signal", "sem", "val", "buf0")

    def __init__(self, eng, fn, is_dma=False, ndma=1):
        self.eng = eng
        self.fn = fn
        self.deps = []
        self.is_dma = is_dma
        self.ndma = ndma
        self.signal = False
        self.sem = None
        self.val = 0
        self.buf0 = None


class SemPool:
    def __init__(self, nc, stack):
        self.nc = nc
        self.stack = stack
        self.eng_sem = {e: None for e in ENGS}
        self.eng_cnt = {e: 0 for e in ENGS}
        self.dma = {"hw": [[stack.enter_context(nc.semaphore("dq%d" % i)), 0] for i in range(N_DMA_SEMS)],
                    "sw": [[stack.enter_context(nc.semaphore("ds%d" % i)), 0] for i in range(N_SWDMA_SEMS)]}
        self.nsem = 0

    def eng_signal(self, e):
        if self.eng_sem[e] is None or self.eng_cnt[e] >= SEM_WINDOW:
            self.nsem += 1
            self.eng_sem[e] = self.stack.enter_context(self.nc.semaphore("c%s%d" % (e, self.nsem)))
            self.eng_cnt[e] = 0
        self.eng_cnt[e] += 1
        return self.eng_sem[e], self.eng_cnt[e]


class Sched:
    def __init__(self, nc, pool):
        self.nc = nc
        self.pool = pool
        self.ops = []

    def _add(self, op, reads, writes):
        deps = op.deps
        for b in reads:
            w = b.writer
            if w is not None and w is not op:
                if w.is_dma or op.is_dma or w.eng != op.eng or op.eng != "pe":
                    deps.append(w)
            b.readers.append(op)
        for b in writes:
            rs = [r for r in b.readers if r is not op]
            if rs:
                for r in rs:
                    if r.is_dma or op.is_dma or r.eng != op.eng:
                        deps.append(r)
            elif b.writer is not None and b.writer is not op:
                w = b.writer
                if w.is_dma or op.is_dma or w.eng != op.eng:
                    deps.append(w)
            b.writer = op
            b.readers = []
        self.ops.append(op)
        return op

    def op(self, eng, fn, reads=(), writes=()):
        return self._add(Op(eng, fn), reads, writes)

    def dma(self, eng, fn, ndma, reads=(), writes=()):
        o = Op(eng, fn, is_dma=True, ndma=ndma)
        o.signal = True
        o.buf0 = (list(writes) + list(reads))[0]
        return self._add(o, reads, writes)

    def flush(self):
        nc, pool = self.nc, self.pool
        ops = self.ops
        for o in ops:
            for d in o.deps:
                d.signal = True
        used = {"hw": 0, "sw": 0}
        for o in ops:
            if o.is_dma:
                b = o.buf0
                kind = "sw" if o.eng == "pool" else "hw"
                if b.dsem is None:
                    b.dkind = kind
                    if b.persist:
                        pool.nsem += 1
                        b.dsem = [pool.stack.enter_context(nc.semaphore("dp%d" % pool.nsem)), 0]
                    else:
                        assert used[kind] < len(pool.dma[kind]), "too many DMA bufs in one phase"
                        b.dsem = pool.dma[kind][used[kind]]
                        used[kind] += 1
                assert b.dkind == kind, "buffer %s mixes software and hardware DMA queues" % b.name
                b.dsem[1] += 16 * o.ndma
                o.sem = b.dsem[0]
                o.val = b.dsem[1]
            elif o.signal:
                o.sem, o.val = pool.eng_signal(o.eng)
        per = {e: [] for e in ENGS}
        for o in ops:
            per[o.eng].append(o)
        finals = {}
        for o in ops:
            if o.is_dma:
                finals[id(o.sem)] = (o.sem, max(o.val, finals.get(id(o.sem), (None, 0))[1]))

        def emit(e_name, e):
            waited = {}
            for o in per[e_name]:
                need = {}
                for d in o.deps:
                    k = id(d.sem)
                    if waited.get(k, 0) >= d.val:
                        continue
                    if k not in need or need[k][1] < d.val:
                        need[k] = (d.sem, d.val)
                for k, (s, v) in need.items():
                    e.wait_ge(s, v)
                    waited[k] = v
                r = o.fn(e)
                if o.is_dma:
                    assert len(r) == o.ndma, (len(r), o.ndma)
                    for ins in r:
                        ins.then_inc(o.sem, 16)
                elif o.signal:
                    r.then_inc(o.sem, 1)
            if e_name == "dve":
                for k, (s, v) in finals.items():
                    if waited.get(k, 0) < v:
                        e.wait_ge(s, v)

        with nc.Block() as block:
            @block.tensor
            def _(e):
                emit("pe", e)

            @block.scalar
            def _(e):
                emit("act", e)

            @block.vector
            def _(e):
                emit("dve", e)

            @block.gpsimd
            def _(e):
                emit("pool", e)

            @block.sync
            def _(e):
                emit("sp", e)
        self.ops = []


def build_nc(upto=99, debug=False):
    nc = bass.Bass("TRN2", target_bir_lowering=False)
    skind = "ExternalOutput" if debug else "Internal"

    def din(name, shape, dt=F32):
        return nc.dram_tensor(name, list(shape), dt, kind="ExternalInput").ap()

    x_own = din("x_own", [NT, D])
    x_prev = din("x_prev", [NT, D])
    c_col = din("c_col", [128, KC])
    pv_in = din("pv", [128, 1])
    w_ada = din("w_ada", [D, 6 * D])
    b_ada = din("b_ada", [1, 6 * D])
    w_ada_f = din("w_ada_f", [D, 2 * D])
    b_ada_f = din("b_ada_f", [1, 2 * D])
    g_mix_col = din("g_mix_col", [128, KC])
    w_in = din("w_in", [D, IN_COLS])
    conv_w_col = din("conv_w_col", [128, 8, 3])
    w_uk = din("w_uk", [8, 512, 128])
    w_uv = din("w_uv", [8, 512, 128])
    kv_g = din("kv_g", [1, 512])
    g_conv_col = din("g_conv_col", [128, 8])
    g_attn = din("g_attn", [1, 1024])
    w_out = din("w_out", [D, D])
    g_ffn_col = din("g_ffn_col", [128, KC])
    w_r = din("w_r", [D, 36])
    b_r = din("b_r", [1, 36])
    if upto >= 6:
        w_gate = din("w_gate", [NEXP, D, FF])
        w_up = din("w_up", [NEXP, D, FF])
        w_down = din("w_down", [NEXP, FF, D])
    g_final = din("g_final", [1, D])
    ident_bf = din("ident_bf", [128, 128], BF16)
    ident_f = din("ident_f", [128, 128])
    tri_in = din("tri", [128, 128])
    pow2_in = din("pow2", [128, NIT + 2])
    out = nc.dram_tensor("out", [NT, D], F32, kind="ExternalOutput").ap()

    modrow = nc.dram_tensor("modrow", [1, 8 * D], F32, kind=skind).ap()
    projT = nc.dram_tensor("projT", [48, 128, NT], BF16, kind=skind).ap()
    haloT = nc.dram_tensor("haloT", [128, 16, 2], BF16, kind=skind).ap()
    mixT_d = nc.dram_tensor("mixT", [16, 128, NT], BF16, kind=skind).ap()

    with ExitStack() as gs:
        def sb(name, shape, dt=F32, st=gs):
            return st.enter_context(nc.sbuf_tensor(name, list(shape), dt))

        def ps(name, shape, dt=F32, st=gs):
            return st.enter_context(nc.psum_tensor(name, list(shape), dt))

        pool = SemPool(nc, gs)
        idb = sb("idb", [128, 128], BF16)
        idf = sb("idf", [128, 128])
        ones_bf = sb("ones_bf", [128, 128], BF16)
        eps_t = sb("eps_t", [128, 1])
        pv = sb("pv_t", [128, 1])
        pvbias = sb("pvbias", [128, 1])
        cact = sb("cact", [128, KC], BF16)
        kst = ExitStack()
        ckv_tok = sb("ckv_tok", [128, 16, 512], BF16, st=kst)
        ckvT = sb("ckvT", [128, 4, SEQ], BF16, st=kst)
        ikT = sb("ikT", [128, SEQ], BF16, st=kst)
        iw_sb = sb("iw_sb", [128, 8, 16], st=kst)
        S = Sched(nc, pool)
        Bc = Buf("cc", persist=True)
        B_const = Buf("const", persist=True)
        B_ckv = Buf("ckv", persist=True)
        B_ik = Buf("ik", persist=True)
        B_iw = Buf("iw", persist=True)
        B_mod = Buf("modrow", persist=True)
        B_proj = Buf("projT", persist=True)
        B_halo = Buf("haloT", persist=True)
        B_mix = Buf("mixT", persist=True)

        def dma1(eng, o, i, reads=(), writes=(), **kw):
            return S.dma(eng, lambda e: [e.dma_start(out=o, in_=i, **kw)], 1, reads=reads, writes=writes)

        def dmaN(eng, pairs, reads=(), writes=(), **kw):
            return S.dma(eng, lambda e: [e.dma_start(out=o, in_=i, **kw) for (o, i) in pairs], len(pairs),
                         reads=reads, writes=writes)

        def mmg(out_ap, pairs, reads, writes):
            n = len(pairs)

            def fn(e):
                r = None
                for k, (l, rr) in enumerate(pairs):
                    r = e.matmul(out_ap, lhsT=l, rhs=rr, start=(k == 0), stop=(k == n - 1))
                return r
            return S.op("pe", fn, reads=reads, writes=writes)

        def mm_multi(groups, reads, writes):
            def fn(e):
                r = None
                for (o, pairs) in groups:
                    n = len(pairs)
                    for k, (l, rr) in enumerate(pairs):
                        r = e.matmul(o, lhsT=l, rhs=rr, start=(k == 0), stop=(k == n - 1))
                return r
            return S.op("pe", fn, reads=reads, writes=writes)

        def transposes(items, reads, writes):
            def fn(e):
                r = None
                for (o, i) in items:
                    r = e.transpose(o, i, idb[:, :])
                return r
            return S.op("pe", fn, reads=reads, writes=writes)

        def act(out_ap, in_ap, func, reads, writes, **kw):
            return S.op("act", lambda e: e.activation(out=out_ap, in_=in_ap, func=func, **kw),
                        reads=reads, writes=writes)

        def tsc(eng, out_ap, in_ap, s1, s2, op0, op1, reads, writes, **kw):
            if s2 is None and op1 is None:
                return S.op(eng, lambda e: e.tensor_scalar(out_ap, in_ap, s1, None, op0, **kw),
                            reads=reads, writes=writes)
            return S.op(eng, lambda e: e.tensor_scalar(out_ap, in_ap, s1, s2, op0, op1, **kw),
                        reads=reads, writes=writes)

        def tt(eng, out_ap, a, b, op, reads, writes):
            return S.op(eng, lambda e: e.tensor_tensor(out_ap, a, b, op), reads=reads, writes=writes)

        def stt(out_ap, in0, scalar, in1, op0, op1, reads, writes):
            return S.op("dve", lambda e: e.scalar_tensor_tensor(out_ap, in0, scalar, in1, op0, op1),
                        reads=reads, writes=writes)

        def cpy(eng, out_ap, in_ap, reads, writes):
            if eng == "act":
                return act(out_ap, in_ap, AF.Identity, reads, writes)
            return S.op(eng, lambda e: e.tensor_copy(out_ap, in_ap), reads=reads, writes=writes)

        def rstd_from_ssq(rs_ap, ssq_ap, inv_n, buf, reads=()):
            act(rs_ap, ssq_ap, AF.Sqrt, [buf, B_const] + list(reads), [buf], scale=inv_n, bias=eps_t[:, 0:1])
            S.op("dve", lambda e: e.reciprocal(rs_ap, rs_ap), reads=[buf], writes=[buf])

        dma1("sp", idb[:, :], ident_bf[:, :], writes=[B_const])
        dma1("sp", idf[:, :], ident_f[:, :], writes=[B_const])
        dma1("sp", pv[:, :], pv_in[:, :], writes=[B_const])
        S.op("dve", lambda e: e.memset(ones_bf[:, :], 1.0), writes=[B_const])
        S.op("dve", lambda e: e.memset(eps_t[:, :], EPS), writes=[B_const])
        tsc("dve", pvbias[:, :], pv[:, :], -1.0, BIG, ALU.add, ALU.mult, [B_const], [B_const])
        S.flush()

        with ExitStack() as ph:
            cc = sb("cc", [128, KC], st=ph)
            wbuf = [sb("wbuf%d" % i, [128, KC, 1024], BF16, st=ph) for i in range(2)]
            brow = [sb("brow%d" % i, [1, 1024], st=ph) for i in range(2)]
            mrow = [sb("mrow%d" % i, [1, 1024], st=ph) for i in range(2)]
            pm = [ps("pm%d" % i, [1, 512], st=ph) for i in range(2)]
            Bw = [Buf("wbuf0"), Buf("wbuf1")]
            Bb = [Buf("brow0"), Buf("brow1")]
            Bm = [Buf("mrow0"), Buf("mrow1")]
            Bp = [Buf("pm0"), Buf("pm1")]
            dma1("sp", cc[:, :], c_col[:, :], writes=[Bc])
            act(cact[:, :], cc[:, :], AF.Silu, [Bc], [Bc])
            for g in range(4):
                wsrc, bsrc, c0 = w_ada, b_ada, g * 1024
                wv = wsrc[:, c0:c0 + 1024].rearrange("(k p) n -> p k n", p=128)
                i2 = g % 2
                dmaN("pool", [(wbuf[i2][:, 4 * q:4 * q + 4, :], wv[:, 4 * q:4 * q + 4, :]) for q in range(4)],
                     writes=[Bw[i2]])
                dma1("sp", brow[i2][:, :], bsrc[0:1, c0:c0 + 1024], writes=[Bb[i2]])
                for j in range(2):
                    mmg(pm[j][:, :], [(cact[:, k:k + 1], wbuf[i2][:, k, j * 512:(j + 1) * 512]) for k in range(KC)],
                        [Bc, Bw[i2]], [Bp[j]])
                    tt("dve", mrow[i2][:, j * 512:(j + 1) * 512], pm[j][:, :], brow[i2][:, j * 512:(j + 1) * 512],
                       ALU.add, [Bp[j], Bb[i2]], [Bm[i2]])
                dma1("sp", modrow[0:1, g * 1024:(g + 1) * 1024], mrow[i2][:, :], reads=[Bm[i2]], writes=[B_mod])
            S.flush()
        if upto <= 0:
            return nc

        def load_mod_col(dst, off, buf, eng="sp"):
            src = modrow[0:1, off:off + D].rearrange("o (k p) -> p (o k)", p=128)
            return dma1(eng, dst, src, reads=[B_mod], writes=[buf], allow_slow_non_contiguous=True)

        def load_bc(dst, src_row, n, buf, reads=(), eng="sp"):
            return dma1(eng, dst, src_row.partition_broadcast(128).rearrange("p o n -> p (o n)"),
                        reads=list(reads), writes=[buf])

        with ExitStack() as ph:
            A1 = sb("A1", [128, KC], st=ph)
            B1 = sb("B1", [128, KC], st=ph)
            sc1 = sb("sc1", [128, KC], st=ph)
            gmc = sb("gmc", [128, KC], st=ph)
            gkv_bc = sb("gkv_bc", [128, 512], st=ph)
            hT = sb("hT", [128, KC, SEQ], BF16, st=ph)
            xt = [sb("xt%d" % i, [128, D], st=ph) for i in range(2)]
            xn = sb("xn", [128, 4, D], BF16, st=ph)
            junk = sb("junk", [128, D], BF16, st=ph)
            ssq = sb("ssq", [128, 16], st=ph)
            rs = sb("rs", [128, 16], st=ph)
            ssk = sb("ssk", [128, 16], st=ph)
            rsk = sb("rsk", [128, 16], st=ph)
            wkv = sb("wkv", [128, KC, 512], BF16, st=ph)
            wik = sb("wik", [128, KC, 128], BF16, st=ph)
            wiw = sb("wiw", [128, KC, 16], BF16, st=ph)
            wg = [sb("wg%d" % i, [128, KC, 512], BF16, st=ph) for i in range(2)]
            stg = [sb("stg%d" % i, [128, NT], BF16, st=ph) for i in range(2)]
            hstg = [sb("hstg%d" % i, [128, 2], BF16, st=ph) for i in range(2)]
            tp = [ps("tp%d" % i, [128, 512], BF16, st=ph) for i in range(2)]
            pkv = [ps("pkv%d" % i, [128, 512], st=ph) for i in range(2)]
            pik = ps("pik", [128, 512], st=ph)
            pp = [ps("pp%d" % i, [128, 512], st=ph) for i in range(2)]
            ph2 = ps("ph2", [128, 16], st=ph)
            BA = Buf("A1B1")
            Bx = [Buf("xt0"), Buf("xt1")]
            Bxn = [Buf("xn%d" % i) for i in range(4)]
            Bjunk = Buf("junk")
            Bss = Buf("ssq")
            BhT = [Buf("hT%d" % i) for i in range(4)]
            Btp = [Buf("tp0"), Buf("tp1")]
            Bwkv = Buf("wkv")
            Bpkv = [Buf("pkv0"), Buf("pkv1")]
            Bpik = Buf("pik")
            Bwg = [Buf("wg0"), Buf("wg1")]
            Bstg = [Buf("stg0"), Buf("stg1")]
            Bhstg = [Buf("hstg0"), Buf("hstg1")]
            Bpp = [Buf("pp0"), Buf("pp1")]
            Bph2 = Buf("ph2")
            Bssk = Buf("ssk")

            load_mod_col(sc1[:, :], 1 * D, BA, eng="pool")
            load_mod_col(B1[:, :], 0 * D, BA, eng="pool")
            dma1("pool", gmc[:, :], g_mix_col[:, :], writes=[BA])
            stt(A1[:, :], sc1[:, :], 1.0, gmc[:, :], ALU.add, ALU.mult, [BA], [BA])
            load_bc(gkv_bc[:, :], kv_g[0:1, :], 512, BA, eng="pool")
            wv = w_in.rearrange("(k p) n -> p k n", p=128)
            dmaN("pool", [(wkv[:, 8 * q:8 * q + 8, :], wv[:, 8 * q:8 * q + 8, OFF_KV:OFF_KV + 512]) for q in range(2)],
                 writes=[Bwkv])
            dma1("pool", wik[:, :, :], wv[:, :, OFF_IK:OFF_IK + 128], writes=[Bwkv])
            dma1("pool", wiw[:, :, :], wv[:, :, OFF_IW:OFF_IW + 16], writes=[Bwkv])

            def F1(g):
                for t4 in range(4):
                    F1tile(g, t4)

            def F1tile(g, t4):
                xsrc = x_prev if g < 2 else x_own
                if True:
                    t = g * 4 + t4
                    r0 = (t % 8) * 128
                    i2 = t % 2
                    dma1("sp", xt[i2][:, :], xsrc[r0:r0 + 128, :], writes=[Bx[i2]])
                    act(junk[:, :], xt[i2][:, :], AF.Square, [Bx[i2]], [Bjunk, Bss], accum_out=ssq[:, t:t + 1])
                    rstd_from_ssq(rs[:, t:t + 1], ssq[:, t:t + 1], 1.0 / D, Bss)
                    tsc("dve", xn[:, t4, :], xt[i2][:, :], rs[:, t:t + 1], None, ALU.mult, None,
                        [Bx[i2], Bss], [Bxn[t4]])

            def F2(g):
                for k in range(KC):
                    i2 = k % 2
                    transposes([(tp[i2][:, t4 * 128:(t4 + 1) * 128], xn[:, t4, k * 128:(k + 1) * 128]) for t4 in range(4)],
                               Bxn + [B_const], [Btp[i2]])
                    if k % 2 == 0:
                        act(hT[:, k, g * 512:(g + 1) * 512], tp[i2][:, :], AF.Identity, [Btp[i2], BA], [BhT[g]],
                            scale=A1[:, k:k + 1], bias=B1[:, k:k + 1])
                    else:
                        tsc("dve", hT[:, k, g * 512:(g + 1) * 512], tp[i2][:, :], A1[:, k:k + 1], B1[:, k:k + 1],
                            ALU.mult, ALU.add, [Btp[i2], BA], [BhT[g]])

            def Ktile(g, t4):
                if True:
                    t = g * 4 + t4
                    i2 = t % 2
                    mmg(pkv[i2][:, :], [(hT[:, k, t * 128:(t + 1) * 128], wkv[:, k, :]) for k in range(KC)],
                        [BhT[g], Bwkv], [Bpkv[i2]])
                    act(junk[:, 0:512], pkv[i2][:, :], AF.Square, [Bpkv[i2]], [Bjunk, Bssk], accum_out=ssk[:, t:t + 1])
                    rstd_from_ssq(rsk[:, t:t + 1], ssk[:, t:t + 1], 1.0 / 512, Bssk)
                    stt(ckv_tok[:, t, :], pkv[i2][:, :], rsk[:, t:t + 1], gkv_bc[:, :], ALU.mult, ALU.mult,
                        [Bpkv[i2], Bssk, BA], [B_ckv])
                    transposes([(tp[i2][:, rc * 128:(rc + 1) * 128], ckv_tok[:, t, rc * 128:(rc + 1) * 128]) for rc in range(4)],
                               [B_ckv, B_const], [Btp[i2]])
                    cpy("dve", ckvT[:, :, t * 128:(t + 1) * 128], tp[i2][:, :].rearrange("p (r q) -> p r q", r=4),
                        [Btp[i2]], [B_ckv])

            def Kik(g):
                mmg(pik[:, :], [(wik[:, k, :], hT[:, k, g * 512:(g + 1) * 512]) for k in range(KC)],
                    [BhT[g], Bwkv], [Bpik])
                cpy("dve", ikT[:, g * 512:(g + 1) * 512], pik[:, :], [Bpik], [B_ik])

            F1(0)
            F2(0)
            for g in range(4):
                for t4 in range(4):
                    Ktile(g, t4)
                    if g + 1 < 4:
                        F1tile(g + 1, t4)
                Kik(g)
                if g + 1 < 4:
                    F2(g + 1)
            for tt8 in range(8):
                c0 = NT + tt8 * 128
                mmg(ph2[:, :], [(hT[:, k, c0:c0 + 128], wiw[:, k, :]) for k in range(KC)],
                    [BhT[2 + tt8 // 4], Bwkv], [Bph2])
                act(iw_sb[:, tt8, :], ph2[:, :], AF.Identity, [Bph2], [B_iw], scale=IW_SCALE)
            for gi in range(12):
                col0 = 512 * gi if gi < 8 else OFF_IQ + 512 * (gi - 8)
                i2 = gi % 2
                dmaN("pool", [(wg[i2][:, 8 * q:8 * q + 8, :], wv[:, 8 * q:8 * q + 8, col0:col0 + 512]) for q in range(2)],
                     writes=[Bwg[i2]])
                for j in range(4):
                    ch = gi * 4 + j
                    s2 = ch % 2
                    for tg in range(2):
                        mmg(pp[tg][:, :], [(wg[i2][:, k, j * 128:(j + 1) * 128], hT[:, k, NT + tg * 512:NT + (tg + 1) * 512])
                                           for k in range(KC)], [Bwg[i2], BhT[2 + tg]], [Bpp[tg]])
                        cpy("act" if tg == 0 else "dve", stg[s2][:, tg * 512:(tg + 1) * 512], pp[tg][:, :],
                            [Bpp[tg]], [Bstg[s2]])
                    dma1("sp", projT[ch, :, :], stg[s2][:, :], reads=[Bstg[s2]], writes=[B_proj])
                    if 8 <= ch < 24:
                        mmg(ph2[:, 0:2], [(wg[i2][:, k, j * 128:(j + 1) * 128], hT[:, k, NT - 2:NT]) for k in range(KC)],
                            [Bwg[i2], BhT[1]], [Bph2])
                        cpy("dve", hstg[s2][:, :], ph2[:, 0:2], [Bph2], [Bhstg[s2]])
                        dma1("sp", haloT[:, ch - 8, :], hstg[s2][:, :], reads=[Bhstg[s2]], writes=[B_halo])
            S.flush()
        if upto <= 1:
            return nc

        with ExitStack() as ph:
            bT = sb("bT", [128, 8, NT], BF16, st=ph)
            cT = sb("cT", [128, 8, NT], BF16, st=ph)
            xT = sb("xT", [128, 8, NT], BF16, st=ph)
            chal = sb("chal", [128, 8, 2], BF16, st=ph)
            xhal = sb("xhal", [128, 8, 2], BF16, st=ph)
            u = sb("u", [128, 8, NT + 2], st=ph)
            cw = sb("cw", [128, 8, 3], st=ph)
            gcc = sb("gcc", [128, 8], st=ph)
            acc = sb("acc", [128, 8, NT], st=ph)
            ysq = sb("ysq", [128, 8, NT], BF16, st=ph)
            rbc = sb("rbc", [128, NT], st=ph)
            mixc = sb("mixc", [128, 8, NT], BF16, st=ph)
            pss = [ps("pss%d" % i, [128, 512], st=ph) for i in range(2)]
            Bh_, Bcw, Brbc = Buf("hal"), Buf("cw"), Buf("rbc")
            Bb_ = [Buf("bT%d" % j) for j in range(8)]
            Bc_ = [Buf("cT%d" % j) for j in range(8)]
            Bx_ = [Buf("xT%d" % j) for j in range(8)]
            Bu = [Buf("u%d" % j) for j in range(8)]
            Bacc = [Buf("acc%d" % j) for j in range(8)]
            Bysq = [Buf("ysq%d" % j) for j in range(8)]
            Bmixc = [Buf("mixc%d" % j) for j in range(8)]
            Buh = Buf("uhalo")
            Bpss = [Buf("pss0"), Buf("pss1")]
            dma1("sp", chal[:, :, :], haloT[:, 0:8, :], reads=[B_halo], writes=[Bh_])
            dma1("sp", xhal[:, :, :], haloT[:, 8:16, :], reads=[B_halo], writes=[Bh_])
            dma1("sp", cw[:, :, :], conv_w_col[:, :, :], writes=[Bcw])
            dma1("sp", gcc[:, :], g_conv_col[:, :], writes=[Bcw])
            for j in range(8):
                dma1("sp", cT[:, j, :], projT[8 + j, :, :], reads=[B_proj], writes=[Bc_[j]])
                dma1("sp", xT[:, j, :], projT[16 + j, :, :], reads=[B_proj], writes=[Bx_[j]])
                dma1("sp", bT[:, j, :], projT[j, :, :], reads=[B_proj], writes=[Bb_[j]])
            stt(u[:, :, 0:2], chal[:, :, :], pv[:, 0:1], xhal[:, :, :], ALU.mult, ALU.mult, [Bh_, B_const], [Buh])
            for j in range(8):
                tt("dve", u[:, j, 2:NT + 2], cT[:, j, :], xT[:, j, :], ALU.mult, [Bc_[j], Bx_[j]], [Bu[j]])
                tsc("dve", acc[:, j, :], u[:, j, 2:NT + 2], cw[:, j, 2:3], None, ALU.mult, None, [Bu[j], Bcw], [Bacc[j]])
                stt(acc[:, j, :], u[:, j, 1:NT + 1], cw[:, j, 1:2], acc[:, j, :], ALU.mult, ALU.add,
                    [Bu[j], Buh, Bcw, Bacc[j]], [Bacc[j]])
                stt(acc[:, j, :], u[:, j, 0:NT], cw[:, j, 0:1], acc[:, j, :], ALU.mult, ALU.add,
                    [Bu[j], Buh, Bcw, Bacc[j]], [Bacc[j]])
                tt("pool", acc[:, j, :], acc[:, j, :], bT[:, j, :], ALU.mult, [Bacc[j], Bb_[j]], [Bacc[j]])
                act(ysq[:, j, :], acc[:, j, :], AF.Square, [Bacc[j]], [Bysq[j]])
            for tg in range(2):
                mmg(pss[tg][:, :], [(ones_bf[:, :], ysq[:, j, tg * 512:(tg + 1) * 512]) for j in range(8)],
                    Bysq + [B_const], [Bpss[tg]])
                rstd_from_ssq(rbc[:, tg * 512:(tg + 1) * 512], pss[tg][:, :], 1.0 / 1024, Brbc, reads=[Bpss[tg]])
            for j in range(8):
                stt(mixc[:, j, :], acc[:, j, :], gcc[:, j:j + 1], rbc[:, :], ALU.mult, ALU.mult,
                    [Bacc[j], Bcw, Brbc], [Bmixc[j]])
                dma1("sp", mixT_d[j, :, :], mixc[:, j, :], reads=[Bmixc[j]], writes=[B_mix])
            S.flush()
        if upto <= 2:
            return nc

        with ExitStack() as ph:
            wuk_sb = sb("wuk_sb", [128, 8, 4, 128], BF16, st=ph)
            wuv_sb = sb("wuv_sb", [128, 8, 4, 128], BF16, st=ph)
            wukT = sb("wukT", [128, 8, 512], BF16, st=ph)
            gat_bc = sb("gat_bc", [128, 1024], st=ph)
            tri = sb("tri_t", [128, 128], st=ph)
            pow2 = sb("pow2_t", [128, NIT + 2], st=ph)
            qT = [sb("qT%d" % i, [128, 8, 128], BF16, st=ph) for i in range(3)]
            iqT = [sb("iqT%d" % i, [128, 16, 128], BF16, st=ph) for i in range(3)]
            qlat = sb("qlat", [128, 4, 8, 128], BF16, st=ph)
            score = sb("score", [128, SEQ], st=ph)
            cjunk = sb("cjunk", [128, SEQ], BF16, st=ph)
            rl = [sb("rl%d" % i, [128, 512], st=ph) for i in range(2)]
            mxmn = sb("mxmn", [128, 4], st=ph)
            Rs = sb("Rs", [128, NIT + 2], st=ph)
            mid = sb("mid", [128, 1], st=ph)
            cnt = sb("cnt", [128, 1], st=ph)
            tstep = sb("tstep", [128, 1], st=ph)
            lof = sb("lof", [128, 1], st=ph)
            selm = sb("selm", [128, SEQ], BF16, st=ph)
            selT = sb("selT", [128, 16, 128], BF16, st=ph)
            praw = [sb("praw%d" % i, [128, 512], BF16, st=ph) for i in range(2)]
            pT = [sb("pT%d" % i, [128, 16, 512], BF16, st=ph) for i in range(2)]
            olat = sb("olat", [128, 4, 8, 128], BF16, st=ph)
            lnd = sb("lnd", [128, 8], st=ph)
            rden = sb("rden", [128, 8], st=ph)
            ysb = sb("ysb", [128, 1024], st=ph)
            yjunk = sb("yjunk", [128, 1024], BF16, st=ph)
            yss = sb("yss", [128, 4], st=ph)
            yn = sb("yn", [128, 1024], BF16, st=ph)
            mixa = [sb("mixa%d" % i, [128, 8, 128], BF16, st=ph) for i in range(2)]
            pa = [ps("pa%d" % i, [128, 512], st=ph) for i in range(2)]
            pb = [ps("pb%d" % i, [128, 512], st=ph) for i in range(2)]
            ptb = [ps("ptb%d" % i, [128, 512], BF16, st=ph) for i in range(2)]
            pmisc = ps("pmisc", [128, 512], st=ph)
            pden = pmisc[:, 0:8]
            pm2 = pmisc[0:1, 0:MG]
            py = ps("py", [128, 512], st=ph)
            wb2 = [sb("wb2_%d" % i, [128, KC, MG], BF16, st=ph) for i in range(2)]
            brow2 = [sb("brow2_%d" % i, [1, MG], st=ph) for i in range(1)] * 2
            mrow2 = [sb("mrow2_%d" % i, [1, MG], st=ph) for i in range(1)] * 2
            Bwb2 = [Buf("wb2_%d" % i) for i in range(3)]
            Bbrow2 = [Buf("brow2_0")] * 2
            Bmrow2 = [Buf("mrow2_0")] * 2
            Bpy = Buf("py")
            Bw3 = Buf("w3")
            BqT = [Buf("qT%d" % i) for i in range(3)]
            BiqT = [Buf("iqT%d" % i) for i in range(3)]
            Bqlat, Bscore, Bcj, Bmm, Bbis, Bsel, BselT, Bolat, Brden, Bysb, Byn = [
                Buf(n) for n in "qlat score cjunk mxmn bis selm selT olat rden ysb yn".split()]
            BpT = [Buf("pT0"), Buf("pT1")]
            Brl = [Buf("rl0"), Buf("rl1")]
            Bpraw = [Buf("praw0"), Buf("praw1")]
            Bmixa = [Buf("mixa0"), Buf("mixa1")]
            Bpa = [Buf("pa0"), Buf("pa1")]
            Bpb = [Buf("pb0"), Buf("pb1")]
            Bptb = [Buf("ptb0"), Buf("ptb1")]
            Bpden = Buf("pden")
            Bpm2 = Bpden

            dma1("pool", wuk_sb[:, :, :, :], w_uk.rearrange("h (r p) d -> p h r d", p=128), writes=[Bw3])
            dma1("pool", wuv_sb[:, :, :, :], w_uv.rearrange("h (r p) d -> p h r d", p=128), writes=[Bw3])
            Bw3s = Buf("w3s")
            load_bc(gat_bc[:, :], g_attn[0:1, :], 1024, Bw3s)
            dma1("sp", tri[:, :], tri_in[:, :], writes=[Bw3s])
            dma1("sp", pow2[:, :], pow2_in[:, :], writes=[Bw3s])

            def LA(qi):
                q0 = qi * 128
                dma1("sp", iqT[qi % 3][:, :, :], projT[32:48, :, q0:q0 + 128].rearrange("h p t -> p h t"),
                     reads=[B_proj], writes=[BiqT[qi % 3]])

            def LB(qi):
                q0 = qi * 128
                dma1("sp", qT[qi % 3][:, :, :], projT[24:32, :, q0:q0 + 128].rearrange("h p t -> p h t"),
                     reads=[B_proj], writes=[BqT[qi % 3]])

            n_def = (16 * 1024 - 4096) // MG

            def DL(j):
                if j >= n_def:
                    return
                c = 4096 + j * MG
                wsrc, bsrc, c0 = (w_ada, b_ada, c) if c < 12 * 1024 else (w_ada_f, b_ada_f, c - 12 * 1024)
                wv_ = wsrc[:, c0:c0 + MG].rearrange("(k p) n -> p k n", p=128)
                b3 = j % 2
                dmaN("pool", [(wb2[b3][:, 8 * q:8 * q + 8, :], wv_[:, 8 * q:8 * q + 8, :]) for q in range(2)],
                     writes=[Bwb2[b3]])

            def DC(j):
                if j >= n_def:
                    return
                c = 4096 + j * MG
                b3 = j % 2
                bsrc, c0 = (b_ada, c) if c < 12 * 1024 else (b_ada_f, c - 12 * 1024)
                dma1("sp", brow2[b3][:, :], bsrc[0:1, c0:c0 + MG], writes=[Bbrow2[b3]])
                mmg(pm2, [(cact[:, k:k + 1], wb2[b3][:, k, :]) for k in range(KC)], [Bc, Bwb2[b3]], [Bpm2])
                act(mrow2[b3][:, :], pm2, AF.Identity, [Bpm2], [Bmrow2[b3]])
                tt("pool", mrow2[b3][:, :], mrow2[b3][:, :], brow2[b3][:, :], ALU.add, [Bmrow2[b3], Bbrow2[b3]], [Bmrow2[b3]])
                dma1("sp", modrow[0:1, c:c + MG], mrow2[b3][:, :], reads=[Bmrow2[b3]], writes=[B_mod])

            ndef = [0]
            DL(0)
            LA(0)
            LA(1)
            LB(0)
            for h in range(8):
                i2 = h % 2
                transposes([(ptb[i2][:, rc * 128:(rc + 1) * 128], wuk_sb[:, h, rc, :]) for rc in range(4)],
                           [Bw3, B_const], [Bptb[i2]])
                cpy("dve", wukT[:, h, :], ptb[i2][:, :], [Bptb[i2]], [Bw3])

            nmm = [0]

            def A1(qi):
                i3 = qi % 3
                n_s = 9 + qi
                nk = n_s * 128
                chunks = [(0, 512), (512, 512)]
                c0 = 1024
                while c0 < nk:
                    cwid = min(512, nk - c0)
                    chunks.append((c0, cwid))
                    c0 += cwid
                n_idx = 0
                for (c0, cwid) in chunks:
                    for h in range(16):
                        k2 = nmm[0] % 2
                        nmm[0] += 1
                        n_idx += 1
                        mmg(pb[k2][:, 0:cwid], [(iqT[i3][:, h, :], ikT[:, c0:c0 + cwid])], [BiqT[i3], B_ik], [Bpb[k2]])
                        act(rl[k2][:, 0:cwid], pb[k2][:, 0:cwid], AF.Relu, [Bpb[k2]], [Brl[k2]])
                        if h == 0:
                            tsc("dve", score[:, c0:c0 + cwid], rl[k2][:, 0:cwid], iw_sb[:, qi, 0:1], None,
                                ALU.mult, None, [Brl[k2], B_iw], [Bscore])
                        else:
                            stt(score[:, c0:c0 + cwid], rl[k2][:, 0:cwid], iw_sb[:, qi, h:h + 1], score[:, c0:c0 + cwid],
                                ALU.mult, ALU.add, [Brl[k2], B_iw, Bscore], [Bscore])
                S.op("dve", lambda e, nk=nk: e.tensor_reduce(mxmn[:, 0:1], score[:, 0:nk], AX.X, ALU.max),
                     reads=[Bscore], writes=[Bmm])
                S.op("dve", lambda e, nk=nk: e.tensor_reduce(mxmn[:, 1:2], score[:, 0:nk], AX.X, ALU.min),
                     reads=[Bscore], writes=[Bmm])
                tsc("dve", score[:, 0:1024], score[:, 0:1024], pvbias[:, 0:1], None, ALU.add, None,
                    [Bscore, B_const], [Bscore])
                tt("dve", score[:, nk - 128:nk], score[:, nk - 128:nk], tri[:, :], ALU.add, [Bscore, Bw3s], [Bscore])
                tsc("dve", mxmn[:, 2:3], mxmn[:, 0:1], mxmn[:, 1:2], None, ALU.subtract, None, [Bmm], [Bmm])
                tsc("dve", mxmn[:, 2:3], mxmn[:, 2:3], 1.002, 1e-9, ALU.mult, ALU.add, [Bmm], [Bmm])
                tsc("dve", Rs[:, :], pow2[:, :], mxmn[:, 2:3], None, ALU.mult, None, [Bmm, Bw3s], [Bbis])
                tsc("dve", mid[:, :], mxmn[:, 0:1], Rs[:, 0:1], None, ALU.subtract, None, [Bmm, Bbis], [Bbis])
                for it in range(NIT):
                    S.op("dve", lambda e, nk=nk: e.tensor_scalar(cjunk[:, 0:nk], score[:, 0:nk], mid[:, 0:1], None,
                                                                 ALU.is_gt, ALU.add, accum_out=cnt[:, 0:1]),
                         reads=[Bscore, Bbis], writes=[Bcj, Bbis])
                    tsc("dve", tstep[:, :], cnt[:, :], TOPK - 0.5, Rs[:, it:it + 1], ALU.is_ge, ALU.mult,
                        [Bbis], [Bbis])
                    tsc("dve", mid[:, :], mid[:, :], tstep[:, 0:1], Rs[:, it + 1:it + 2], ALU.add, ALU.subtract,
                        [Bbis], [Bbis])
                tsc("dve", lof[:, :], mid[:, :], Rs[:, NIT:NIT + 1], None, ALU.subtract, None, [Bbis], [Bbis])
                tsc("dve", selm[:, 0:nk], score[:, 0:nk], lof[:, 0:1], None, ALU.is_gt, None, [Bscore, Bbis], [Bsel])

            def A2(qi):
                n_s = 9 + qi
                for s4 in range(0, n_s, 4):
                    k2 = (s4 // 4) % 2
                    nn = min(4, n_s - s4)
                    transposes([(ptb[k2][:, a * 128:(a + 1) * 128], selm[:, (s4 + a) * 128:(s4 + a + 1) * 128])
                                for a in range(nn)], [Bsel, B_const], [Bptb[k2]])
                    cpy("act", selT[:, s4:s4 + nn, :], ptb[k2][:, 0:nn * 128].rearrange("p (a q) -> p a q", a=nn),
                        [Bptb[k2]], [BselT])

            def Bst(qi):
                i2 = qi % 2
                i3 = qi % 3
                n_s = 9 + qi
                q0 = qi * 128
                for rc in range(4):
                    for hg in range(2):
                        k2 = (rc * 2 + hg) % 2
                        mm_multi([(pa[k2][:, hh * 128:(hh + 1) * 128],
                                   [(wukT[:, hg * 4 + hh, rc * 128:(rc + 1) * 128], qT[i3][:, hg * 4 + hh, :])])
                                  for hh in range(4)], [Bw3, BqT[i3]], [Bpa[k2]])
                        cpy("act", qlat[:, rc, hg * 4:(hg + 1) * 4, :],
                            pa[k2][:, :].rearrange("p (h q) -> p h q", h=4), [Bpa[k2]], [Bqlat])
                n_it = 0
                for hg in range(2):
                    for st in range(n_s):
                        k2 = st % 2
                        n_it += 1
                        if n_it % 6 == 0 and n_it <= 18:
                            DL(ndef[0] + 1)
                            DC(ndef[0])
                            ndef[0] += 1
                        mmg(pa[k2][:, :], [(ckvT[:, rc, st * 128:(st + 1) * 128], qlat[:, rc, hg * 4:(hg + 1) * 4, :])
                                           for rc in range(4)], [B_ckv, Bqlat], [Bpa[k2]])
                        act(praw[k2][:, :], pa[k2][:, :], AF.Exp, [Bpa[k2]], [Bpraw[k2]], scale=ATT_SCALE)
                        tt("pool", pT[hg][:, st, :].rearrange("p (h q) -> p h q", h=4),
                           praw[k2][:, :].rearrange("p (h q) -> p h q", h=4),
                           selT[:, st, :].unsqueeze(1).to_broadcast([128, 4, 128]), ALU.mult,
                           [Bpraw[k2], BselT], [BpT[hg]])
                for hg in range(2):
                    for rc in range(4):
                        k2 = rc % 2
                        mmg(pb[k2][:, :], [(ckv_tok[:, st, rc * 128:(rc + 1) * 128], pT[hg][:, st, :]) for st in range(n_s)],
                            [B_ckv, BpT[hg]], [Bpb[k2]])
                        cpy("act", olat[:, rc, hg * 4:(hg + 1) * 4, :],
                            pb[k2][:, :].rearrange("p (h q) -> p h q", h=4), [Bpb[k2]], [Bolat])
                    mm_multi([(pden[:, hg * 4 + hh:hg * 4 + hh + 1],
                               [(pT[hg][:, st, hh * 128:(hh + 1) * 128], ones_bf[:, 0:1]) for st in range(n_s)])
                              for hh in range(4)], [BpT[hg], B_const], [Bpden])
                    mm_multi([(py[:, hh * 128:(hh + 1) * 128],
                               [(olat[:, rc, hg * 4 + hh, :], wuv_sb[:, hg * 4 + hh, rc, :]) for rc in range(4)])
                              for hh in range(4)], [Bolat, Bw3], [Bpy])
                    act(lnd[:, hg * 4:(hg + 1) * 4], pden[:, hg * 4:(hg + 1) * 4], AF.Ln, [Bpden], [Brden])
                    act(rden[:, hg * 4:(hg + 1) * 4], lnd[:, hg * 4:(hg + 1) * 4], AF.Exp, [Brden], [Brden], scale=-1.0)
                    for hh in range(4):
                        h = hg * 4 + hh
                        act(ysb[:, h * 128:(h + 1) * 128], py[:, hh * 128:(hh + 1) * 128], AF.Identity,
                            [Bpy, Brden], [Bysb], scale=rden[:, h:h + 1])
            def Bepi(qi):
                i2 = qi % 2
                q0 = qi * 128
                act(yjunk[:, :], ysb[:, :], AF.Square, [Bysb], [Byn], accum_out=yss[:, 0:1])
                act(yss[:, 1:2], yss[:, 0:1], AF.Ln, [Byn, B_const], [Byn], scale=1.0 / 1024, bias=eps_t[:, 0:1])
                act(yss[:, 2:3], yss[:, 1:2], AF.Exp, [Byn], [Byn], scale=-0.5)
                act(ysb[:, :], ysb[:, :], AF.Identity, [Bysb, Byn], [Bysb], scale=yss[:, 2:3])
                tt("pool", yn[:, :], ysb[:, :], gat_bc[:, :], ALU.mult, [Bysb, Bw3s], [Byn])
                for c4 in range(2):
                    transposes([(ptb[c4][:, a * 128:(a + 1) * 128], yn[:, (c4 * 4 + a) * 128:(c4 * 4 + a + 1) * 128])
                                for a in range(4)], [Byn, B_const], [Bptb[c4]])
                    cpy("act", mixa[i2][:, c4 * 4:(c4 + 1) * 4, :],
                        ptb[c4][:, :].rearrange("p (a q) -> p a q", a=4), [Bptb[c4]], [Bmixa[i2]])
                dma1("sp", mixT_d[8:16, :, q0:q0 + 128].rearrange("c p t -> p c t"), mixa[i2][:, :, :],
                     reads=[Bmixa[i2]], writes=[B_mix])

            A1(0)
            A2(0)
            for qi in range(8):
                if qi + 2 < 8:
                    LA(qi + 2)
                if qi + 1 < 8:
                    LB(qi + 1)
                    A1(qi + 1)
                if qi > 0:
                    Bepi(qi - 1)
                Bst(qi)
                if qi + 1 < 8:
                    A2(qi + 1)
            Bepi(7)
            while ndef[0] < n_def:
                DL(ndef[0] + 1)
                DC(ndef[0])
                ndef[0] += 1
            S.flush()
        if upto <= 3:
            return nc

        kst.close()
        xr = sb("xr", [128, 8, D])
        h2T = sb("h2T", [128, KC, NT], BF16)
        comb_tok = sb("comb_tok", [128, 8, NEXP])
        Bxr = [Buf("xr%d" % i, persist=True) for i in range(8)]
        Bh2 = Buf("h2T", persist=True)
        BcombT = Buf("comb_tok", persist=True)

        ph4 = ExitStack()
        if True:
            ph = ph4
            mixT = sb("mixT_s", [128, 16, NT], BF16, st=ph)
            gt1 = sb("gt1", [128, D], st=ph)
            wo = [sb("wo%d" % i, [128, KC, 512], BF16, st=ph) for i in range(2)]
            tmp = [sb("tmp%d" % i, [128, 512], st=ph) for i in range(2)]
            pw = [ps("pw%d" % i, [128, 512], st=ph) for i in range(2)]
            BmixS, Bgt1 = Buf("mixS"), Buf("gt1")
            Bwo = [Buf("wo0"), Buf("wo1")]
            Btmp = [Buf("tmp0"), Buf("tmp1")]
            Bpw = [Buf("pw0"), Buf("pw1")]
            dma1("sp", mixT[:, :, :], mixT_d.rearrange("c p t -> p c t"), reads=[B_mix], writes=[BmixS])
            load_bc(gt1[:, :], modrow[0:1, 2 * D:3 * D], D, Bgt1, reads=[B_mod])
            for t8 in range(8):
                dma1("sp", xr[:, t8, :], x_own[t8 * 128:(t8 + 1) * 128, :], writes=[Bxr[t8]])
            wov = w_out.rearrange("(k p) n -> p k n", p=128)
            n = 0
            for ng in range(4):
                i2 = ng % 2
                dmaN("pool", [(wo[i2][:, 8 * q:8 * q + 8, :], wov[:, 8 * q:8 * q + 8, ng * 512:(ng + 1) * 512])
                              for q in range(2)], writes=[Bwo[i2]])
                for t8 in range(8):
                    k2 = n % 2
                    n += 1
                    mmg(pw[k2][:, :], [(mixT[:, k, t8 * 128:(t8 + 1) * 128], wo[i2][:, k, :]) for k in range(KC)],
                        [BmixS, Bwo[i2]], [Bpw[k2]])
                    tt("dve", tmp[k2][:, :], pw[k2][:, :], gt1[:, ng * 512:(ng + 1) * 512], ALU.mult,
                       [Bpw[k2], Bgt1], [Btmp[k2]])
                    tt("pool", xr[:, t8, ng * 512:(ng + 1) * 512], xr[:, t8, ng * 512:(ng + 1) * 512], tmp[k2][:, :],
                       ALU.add, [Btmp[k2], Bxr[t8]], [Bxr[t8]])
        if upto <= 4:
            S.flush()
            ph4.close()
            if debug:
                for t8 in range(8):
                    dma1("sp", out[t8 * 128:(t8 + 1) * 128, :], xr[:, t8, :], reads=[Bxr[t8]])
                S.flush()
            return nc

        with ExitStack() as ph:
            A2 = sb("A2", [128, KC], st=ph)
            B2 = sb("B2", [128, KC], st=ph)
            sc2 = sb("sc2", [128, KC], st=ph)
            gfc = sb("gfc", [128, KC], st=ph)
            xn = sb("xn5", [128, 4, D], BF16, st=ph)
            junk = sb("junk5", [128, D], BF16, st=ph)
            ssq = sb("ssq5", [128, 8], st=ph)
            rs = sb("rs5", [128, 8], st=ph)
            wr = sb("wr", [128, KC, 36], BF16, st=ph)
            br = sb("br", [128, 36], st=ph)
            lg = sb("lg", [128, 36], st=ph)
            sm = sb("sm", [128, 16], st=ph)
            oh = sb("oh", [128, 4], st=ph)
            ge = sb("ge", [128, 4], st=ph)
            em = sb("em", [128, 32], st=ph)
            pe_ = sb("pe_", [128, 32], st=ph)
            nm1 = sb("nm1", [128, 32], st=ph)
            pe2 = sb("pe2", [128, 32], st=ph)
            sel2 = sb("sel2", [128, 32], st=ph)
            tp = [ps("tp5%d" % i, [128, 512], BF16, st=ph) for i in range(2)]
            pr = ps("pr", [128, 36], st=ph)
            BA = Buf("A2B2")
            Bxn = [Buf("xn5%d" % i) for i in range(4)]
            Bj, Bss, Bwr, Blg, Bsm, Bcomb, Bpr, Bpct = [Buf(n) for n in "junk ss wr lg sm comb pr pct".split()]
            Btp = [Buf("tp0"), Buf("tp1")]
            load_mod_col(sc2[:, :], 4 * D, BA)
            load_mod_col(B2[:, :], 3 * D, BA)
            dma1("sp", gfc[:, :], g_ffn_col[:, :], writes=[BA])
            stt(A2[:, :], sc2[:, :], 1.0, gfc[:, :], ALU.add, ALU.mult, [BA], [BA])
            dma1("pool", wr[:, :, :], w_r.rearrange("(k p) n -> p k n", p=128), writes=[Bwr])
            Bbr = Buf("br")
            load_bc(br[:, :], b_r[0:1, :], 36, Bbr)
            def G1(g):
                for t4 in range(4):
                    t = g * 4 + t4
                    act(junk[:, :], xr[:, t, :], AF.Square, [Bxr[t]], [Bj, Bss], accum_out=ssq[:, t:t + 1])
                    rstd_from_ssq(rs[:, t:t + 1], ssq[:, t:t + 1], 1.0 / D, Bss)
                    tsc("dve", xn[:, t4, :], xr[:, t, :], rs[:, t:t + 1], None, ALU.mult, None, [Bxr[t], Bss], [Bxn[t4]])

            def G2(g):
                for k in range(KC):
                    i2 = k % 2
                    transposes([(tp[i2][:, t4 * 128:(t4 + 1) * 128], xn[:, t4, k * 128:(k + 1) * 128]) for t4 in range(4)],
                               Bxn + [B_const], [Btp[i2]])
                    if k % 2 == 0:
                        act(h2T[:, k, g * 512:(g + 1) * 512], tp[i2][:, :], AF.Identity, [Btp[i2], BA], [Bh2g[g]],
                            scale=A2[:, k:k + 1], bias=B2[:, k:k + 1])
                    else:
                        tsc("dve", h2T[:, k, g * 512:(g + 1) * 512], tp[i2][:, :], A2[:, k:k + 1], B2[:, k:k + 1],
                            ALU.mult, ALU.add, [Btp[i2], BA], [Bh2g[g]])

            Bh2g = [Buf("h2Tg0"), Buf("h2Tg1")]

            def router(t):
                    mmg(pr[:, :], [(h2T[:, k, t * 128:(t + 1) * 128], wr[:, k, :]) for k in range(KC)], [Bh2g[t // 4], Bwr], [Bpr])
                    tt("dve", lg[:, :], pr[:, :], br[:, :], ALU.add, [Bpr, Bbr], [Blg])
                    S.op("dve", lambda e: e.tensor_reduce(sm[:, 0:1], lg[:, 0:4], AX.X, ALU.max), reads=[Blg], writes=[Bsm])
                    tsc("dve", sm[:, 1:2], sm[:, 0:1], -1.0, None, ALU.mult, None, [Bsm], [Bsm])
                    act(ge[:, :], lg[:, 0:4], AF.Exp, [Blg, Bsm], [Bsm], bias=sm[:, 1:2], accum_out=sm[:, 2:3])
                    tsc("dve", oh[:, :], lg[:, 0:4], sm[:, 0:1], None, ALU.is_ge, None, [Blg, Bsm], [Bsm])
                    tsc("dve", oh[:, :], oh[:, :], -1.0, BIG, ALU.add, ALU.mult, [Bsm], [Bsm])
                    for g4 in range(4):
                        tsc("dve", em[:, g4 * 8:(g4 + 1) * 8], lg[:, 4 + g4 * 8:4 + (g4 + 1) * 8], oh[:, g4:g4 + 1], None,
                            ALU.add, None, [Blg, Bsm], [Bsm])
                    S.op("dve", lambda e: e.tensor_reduce(sm[:, 3:4], em[:, :], AX.X, ALU.max), reads=[Bsm], writes=[Bsm])
                    tsc("dve", sm[:, 4:5], sm[:, 3:4], -1.0, None, ALU.mult, None, [Bsm], [Bsm])
                    act(pe_[:, :], em[:, :], AF.Exp, [Bsm], [Bsm], bias=sm[:, 4:5])
                    S.op("dve", lambda e: e.tensor_reduce(sm[:, 5:6], pe_[:, :], AX.X, ALU.max), reads=[Bsm], writes=[Bsm])
                    tsc("dve", nm1[:, :], pe_[:, :], sm[:, 5:6], None, ALU.is_lt, None, [Bsm], [Bsm])
                    tt("dve", pe2[:, :], pe_[:, :], nm1[:, :], ALU.mult, [Bsm], [Bsm])
                    S.op("dve", lambda e: e.tensor_reduce(sm[:, 6:7], pe2[:, :], AX.X, ALU.max), reads=[Bsm], writes=[Bsm])
                    tsc("dve", sel2[:, :], pe_[:, :], sm[:, 6:7], None, ALU.is_ge, None, [Bsm], [Bsm])
                    tt("dve", sm[:, 7:8], sm[:, 5:6], sm[:, 6:7], ALU.add, [Bsm], [Bsm])
                    tt("dve", sm[:, 7:8], sm[:, 7:8], sm[:, 2:3], ALU.mult, [Bsm], [Bsm])
                    S.op("dve", lambda e: e.reciprocal(sm[:, 8:9], sm[:, 7:8]), reads=[Bsm], writes=[Bsm])
                    stt(comb_tok[:, t, :], pe_[:, :], sm[:, 8:9], sel2[:, :], ALU.mult, ALU.mult, [Bsm], [BcombT])
            G1(0)
            G2(0)
            G1(1)
            for t in range(4):
                router(t)
            G2(1)
            for t in range(4, 8):
                router(t)
            S.op("dve", lambda e: e.memset(sm[:, 15:16], 0.0), reads=[Bh2g[0], Bh2g[1]], writes=[Bh2])
            S.flush()
        ph4.close()
        if upto <= 5:
            return nc

        with ExitStack() as ph:
            gt2 = sb("gt2", [128, D], st=ph)
            wge = [sb("wge%d" % i, [128, KC, FF], BF16, st=ph) for i in range(2)]
            wue = [sb("wue%d" % i, [128, KC, FF], BF16, st=ph) for i in range(2)]
            wde = [sb("wde%d" % i, [128, 4, D], BF16, st=ph) for i in range(1)]
            sg = [sb("sg%d" % i, [128, 512], BF16, st=ph) for i in range(2)]
            hid = sb("hid", [128, 4, NT], BF16, st=ph)
            pA = [ps("pA%d" % i, [128, 512], st=ph) for i in range(2)]
            pU = [ps("pU%d" % i, [128, 512], st=ph) for i in range(2)]
            pD = [ps("pD%d" % i, [128, 512], st=ph) for i in range(4)]
            Bgt2 = Buf("gt2")
            Bwge = [Buf("wge0"), Buf("wge1")]
            Bwue = [Buf("wue0"), Buf("wue1")]
            Bwde = [Buf("wde_a"), Buf("wde_b")]
            Bsg = [Buf("sg0"), Buf("sg1")]
            Bhid = Buf("hid")
            BpA = [Buf("pA0"), Buf("pA1")]
            BpU = [Buf("pU0"), Buf("pU1")]
            BpD = [Buf("pD%d" % i) for i in range(4)]
            load_bc(gt2[:, :], modrow[0:1, 5 * D:6 * D], D, Bgt2, reads=[B_mod])

            def load_gu(e_):
                i2 = e_ % 2
                gv = w_gate[e_].rearrange("(k p) f -> p k f", p=128)
                uv = w_up[e_].rearrange("(k p) f -> p k f", p=128)
                dmaN("pool", [(wge[i2][:, 8 * q:8 * q + 8, :], gv[:, 8 * q:8 * q + 8, :]) for q in range(2)],
                     writes=[Bwge[i2]])
                dmaN("pool", [(wue[i2][:, 8 * q:8 * q + 8, :], uv[:, 8 * q:8 * q + 8, :]) for q in range(2)],
                     writes=[Bwue[i2]])

            def load_d_half(e_, hf):
                dv = w_down[e_].rearrange("(c p) d -> p c d", p=128)
                c0 = hf * 1024
                dmaN("pool", [(wde[0][:, 2 * q:2 * q + 2, c0:c0 + 1024], dv[:, 2 * q:2 * q + 2, c0:c0 + 1024])
                              for q in range(2)], writes=[Bwde[hf]])

            def prescale_half(hf):
                c0 = hf * 1024
                for fc in range(4):
                    tt("dve", wde[0][:, fc, c0:c0 + 1024], wde[0][:, fc, c0:c0 + 1024], gt2[:, c0:c0 + 1024], ALU.mult,
                       [Bwde[hf], Bgt2], [Bwde[hf]])

            load_gu(0)
            load_d_half(0, 0)
            load_d_half(0, 1)
            prescale_half(0)
            prescale_half(1)
            nd = 0
            for e_ in range(NEXP):
                i2 = e_ % 2
                if e_ + 1 < NEXP:
                    load_gu(e_ + 1)
                n = 0
                for tg in range(2):
                    for fc in range(4):
                        k2 = n % 2
                        n += 1
                        mmg(pA[k2][:, :], [(wge[i2][:, k, fc * 128:(fc + 1) * 128], h2T[:, k, tg * 512:(tg + 1) * 512])
                                           for k in range(KC)], [Bwge[i2], Bh2], [BpA[k2]])
                        mmg(pU[k2][:, :], [(wue[i2][:, k, fc * 128:(fc + 1) * 128], h2T[:, k, tg * 512:(tg + 1) * 512])
                                           for k in range(KC)], [Bwue[i2], Bh2], [BpU[k2]])
                        act(sg[k2][:, :], pA[k2][:, :], AF.Silu, [BpA[k2]], [Bsg[k2]])
                        tt("dve", hid[:, fc, tg * 512:(tg + 1) * 512], pU[k2][:, :], sg[k2][:, :], ALU.mult,
                           [BpU[k2], Bsg[k2]], [Bhid])
                        if e_ > 0 and n == 4:
                            prescale_half(0)
                        if e_ > 0 and n == 6:
                            prescale_half(1)
                for ng in range(4):
                    for t8 in range(8):
                        k2 = nd % 4
                        nd += 1
                        mmg(pD[k2][:, :], [(hid[:, fc, t8 * 128:(t8 + 1) * 128], wde[0][:, fc, ng * 512:(ng + 1) * 512])
                                           for fc in range(4)], [Bhid, Bwde[ng // 2]], [BpD[k2]])
                        stt(xr[:, t8, ng * 512:(ng + 1) * 512], pD[k2][:, :], comb_tok[:, t8, e_:e_ + 1],
                            xr[:, t8, ng * 512:(ng + 1) * 512], ALU.mult, ALU.add,
                            [BpD[k2], Bxr[t8], BcombT], [Bxr[t8]])
                    if ng % 2 == 1 and e_ + 1 < NEXP:
                        load_d_half(e_ + 1, ng // 2)
            S.flush()
        if upto <= 6:
            return nc

        with ExitStack() as ph:
            Af = sb("Af", [128, D], st=ph)
            gfb = sb("gfb", [128, D], st=ph)
            shf = sb("shf", [128, D], st=ph)
            junk = sb("junk7", [128, D], BF16, st=ph)
            ssq = sb("ssq7", [128, 8], st=ph)
            rs = sb("rs7", [128, 8], st=ph)
            ob = [sb("ob%d" % i, [128, D], st=ph) for i in range(2)]
            BAf, Bj, Bss = Buf("Af"), Buf("junk"), Buf("ss")
            Bob = [Buf("ob0"), Buf("ob1")]
            load_bc(Af[:, :], modrow[0:1, 7 * D:8 * D], D, BAf, reads=[B_mod])
            load_bc(shf[:, :], modrow[0:1, 6 * D:7 * D], D, BAf, reads=[B_mod])
            load_bc(gfb[:, :], g_final[0:1, :], D, BAf)
            stt(Af[:, :], Af[:, :], 1.0, gfb[:, :], ALU.add, ALU.mult, [BAf], [BAf])
            for t in range(8):
                i2 = t % 2
                act(junk[:, :], xr[:, t, :], AF.Square, [Bxr[t]], [Bj, Bss], accum_out=ssq[:, t:t + 1])
                rstd_from_ssq(rs[:, t:t + 1], ssq[:, t:t + 1], 1.0 / D, Bss)
                stt(ob[i2][:, :], xr[:, t, :], rs[:, t:t + 1], Af[:, :], ALU.mult, ALU.mult, [Bxr[t], Bss, BAf], [Bob[i2]])
                tt("pool", ob[i2][:, :], ob[i2][:, :], shf[:, :], ALU.add, [Bob[i2], BAf], [Bob[i2]])
                dma1("sp", out[t * 128:(t + 1) * 128, :], ob[i2][:, :], reads=[Bob[i2]])
            S.flush()
    return nc


def make_in_maps(x, c, w_ada, b_ada, g_mix, w_in, conv_w, w_uk, kv_norm_g, w_uv, g_conv_out, g_attn_out,
                 w_out, g_ffn, w_rg, b_rg, w_re, b_re, w_gate, w_up, w_down, w_ada_f, b_ada_f, g_final):
    f = np.float32
    a = lambda v: np.ascontiguousarray(np.asarray(v, dtype=f))
    col = lambda v: a(np.asarray(v).reshape(-1, 128).T)
    x = a(x)
    c = a(c)
    tri = np.where(np.arange(128)[None, :] <= np.arange(128)[:, None], 0.0, -BIG).astype(f)
    pow2 = np.broadcast_to((2.0 ** -(np.arange(NIT + 2) + 1.0)).astype(f)[None, :], (128, NIT + 2))
    shared = {
        "w_ada": a(w_ada[0]), "b_ada": a(b_ada[0]).reshape(1, -1),
        "w_ada_f": a(w_ada_f), "b_ada_f": a(b_ada_f).reshape(1, -1),
        "g_mix_col": col(g_mix[0]), "w_in": a(w_in[0]),
        "conv_w_col": a(np.asarray(conv_w[0]).reshape(3, 8, 128).transpose(2, 1, 0)),
        "w_uk": a(w_uk[0]), "w_uv": a(w_uv[0]), "kv_g": a(kv_norm_g[0]).reshape(1, -1),
        "g_conv_col": col(g_conv_out[0]), "g_attn": a(g_attn_out[0]).reshape(1, -1),
        "w_out": a(w_out[0]), "g_ffn_col": col(g_ffn[0]),
        "w_r": a(np.concatenate([np.asarray(w_rg[0]), np.asarray(w_re[0])], axis=1)),
        "b_r": a(np.concatenate([np.asarray(b_rg[0]), np.asarray(b_re[0])])).reshape(1, -1),
        "w_gate": a(w_gate[0]), "w_up": a(w_up[0]), "w_down": a(w_down[0]),
        "g_final": a(g_final).reshape(1, -1),
        "ident_bf": np.eye(128, dtype=ml_dtypes.bfloat16), "ident_f": np.eye(128, dtype=f),
        "tri": tri, "pow2": a(pow2),
    }
    maps = []
    for core in range(8):
        b, half = core // 2, core % 2
        m = dict(shared)
        m["x_own"] = a(x[b, half * NT:(half + 1) * NT])
        m["x_prev"] = a(x[b, 0:NT])
        m["c_col"] = col(c[b])
        m["pv"] = np.full((128, 1), float(half), dtype=f)
        maps.append(m)
    return maps


_NC_CACHE = {}


def kernel(**inputs):
    maps = make_in_maps(**inputs)
    if "nc" not in _NC_CACHE:
        _NC_CACHE["nc"] = build_nc()
    res = run_bass_kernel_spmd(_NC_CACHE["nc"], maps, core_ids=list(range(8)))
    outp = np.empty((4, SEQ, D), dtype=np.float32)
    for core in range(8):
        b, half = core // 2, core % 2
        outp[b, half * NT:(half + 1) * NT] = res.results[core]["out"]
    return outp
```

```python
import numpy as np
import ml_dtypes
from contextlib import ExitStack
import concourse.bass as bass
import concourse.mybir as mybir
from concourse.bass_utils import run_bass_kernel_spmd

F32 = mybir.dt.float32
BF16 = mybir.dt.bfloat16
AF = mybir.ActivationFunctionType
ALU = mybir.AluOpType
AX = mybir.AxisListType

D = 2048
SEQ = 2048
NT = 1024
KC = 16
IN_COLS = 6800
OFF_KV, OFF_IQ, OFF_IK, OFF_IW = 4096, 4608, 6656, 6784
NEXP = 32
FF = 512
TOPK = 256
NIT = 24
MG = 512
BIG = 1.0e30
EPS = 1e-6
ATT_SCALE = 128 ** -0.5
IW_SCALE = (16 ** -0.5) * (128 ** -0.5)

ENGS = ("pe", "act", "dve", "pool", "sp")
SEM_WINDOW = 3000
N_DMA_SEMS = 34
N_SWDMA_SEMS = 10


class Buf:
    __slots__ = ("name", "writer", "readers", "dsem", "dcount", "persist", "dkind")

    def __init__(self, name, persist=False):
        self.name = name
        self.writer = None
        self.readers = []
        self.dsem = None
        self.dcount = 0
        self.persist = persist
        self.dkind = None


class Op:
    __slots__ = ("eng", "fn", "deps", "is_dma", "ndma", "signal", "sem", "val", "buf0")

    def __init__(self, eng, fn, is_dma=False, ndma=1):
        self.eng = eng
        self.fn = fn
        self.deps = []
        self.is_dma = is_dma
        self.ndma = ndma
        self.signal = False
        self.sem = None
        self.val = 0
        self.buf0 = None


class SemPool:
    def __init__(self, nc, stack):
        self.nc = nc
        self.stack = stack
        self.eng_sem = {e: None for e in ENGS}
        self.eng_cnt = {e: 0 for e in ENGS}
        self.dma = {"hw": [[stack.enter_context(nc.semaphore("dq%d" % i)), 0] for i in range(N_DMA_SEMS)],
                    "sw": [[stack.enter_context(nc.semaphore("ds%d" % i)), 0] for i in range(N_SWDMA_SEMS)]}
        self.nsem = 0

    def eng_signal(self, e):
        if self.eng_sem[e] is None or self.eng_cnt[e] >= SEM_WINDOW:
            self.nsem += 1
            self.eng_sem[e] = self.stack.enter_context(self.nc.semaphore("c%s%d" % (e, self.nsem)))
            self.eng_cnt[e] = 0
        self.eng_cnt[e] += 1
        return self.eng_sem[e], self.eng_cnt[e]


class Sched:
    def __init__(self, nc, pool):
        self.nc = nc
        self.pool = pool
        self.ops = []

    def _add(self, op, reads, writes):
        deps = op.deps
        for b in reads:
            w = b.writer
            if w is not None and w is not op:
                if w.is_dma or op.is_dma or w.eng != op.eng or op.eng != "pe":
                    deps.append(w)
            b.readers.append(op)
        for b in writes:
            rs = [r for r in b.readers if r is not op]
            if rs:
                for r in rs:
                    if r.is_dma or op.is_dma or r.eng != op.eng:
                        deps.append(r)
            elif b.writer is not None and b.writer is not op:
                w = b.writer
                if w.is_dma or op.is_dma or w.eng != op.eng:
                    deps.append(w)
            b.writer = op
            b.readers = []
        self.ops.append(op)
        return op

    def op(self, eng, fn, reads=(), writes=()):
        return self._add(Op(eng, fn), reads, writes)

    def dma(self, eng, fn, ndma, reads=(), writes=()):
        o = Op(eng, fn, is_dma=True, ndma=ndma)
        o.signal = True
        o.buf0 = (list(writes) + list(reads))[0]
        return self._add(o, reads, writes)

    def flush(self):
        nc, pool = self.nc, self.pool
        ops = self.ops
        for o in ops:
            for d in o.deps:
                d.signal = True
        used = {"hw": 0, "sw": 0}
        for o in ops:
            if o.is_dma:
                b = o.buf0
                kind = "sw" if o.eng == "pool" else "hw"
                if b.dsem is None:
                    b.dkind = kind
                    if b.persist:
                        pool.nsem += 1
                        b.dsem = [pool.stack.enter_context(nc.semaphore("dp%d" % pool.nsem)), 0]
                    else:
                        assert used[kind] < len(pool.dma[kind]), "too many DMA bufs in one phase"
                        b.dsem = pool.dma[kind][used[kind]]
                        used[kind] += 1
                assert b.dkind == kind, "buffer %s mixes software and hardware DMA queues" % b.name
                b.dsem[1] += 16 * o.ndma
                o.sem = b.dsem[0]
                o.val = b.dsem[1]
            elif o.signal:
                o.sem, o.val = pool.eng_signal(o.eng)
        per = {e: [] for e in ENGS}
        for o in ops:
            per[o.eng].append(o)
        finals = {}
        for o in ops:
            if o.is_dma:
                finals[id(o.sem)] = (o.sem, max(o.val, finals.get(id(o.sem), (None, 0))[1]))

        def emit(e_name, e):
            waited = {}
            for o in per[e_name]:
                need = {}
                for d in o.deps:
                    k = id(d.sem)
                    if waited.get(k, 0) >= d.val:
                        continue
                    if k not in need or need[k][1] < d.val:
                        need[k] = (d.sem, d.val)
                for k, (s, v) in need.items():
                    e.wait_ge(s, v)
                    waited[k] = v
                r = o.fn(e)
                if o.is_dma:
                    assert len(r) == o.ndma, (len(r), o.ndma)
                    for ins in r:
                        ins.then_inc(o.sem, 16)
                elif o.signal:
                    r.then_inc(o.sem, 1)
            if e_name == "dve":
                for k, (s, v) in finals.items():
                    if waited.get(k, 0) < v:
                        e.wait_ge(s, v)

        with nc.Block() as block:
            @block.tensor
            def _(e):
                emit("pe", e)

            @block.scalar
            def _(e):
                emit("act", e)

            @block.vector
            def _(e):
                emit("dve", e)

            @block.gpsimd
            def _(e):
                emit("pool", e)

            @block.sync
            def _(e):
                emit("sp", e)
        self.ops = []


def build_nc(upto=99, debug=False):
    nc = bass.Bass("TRN2", target_bir_lowering=False)
    skind = "ExternalOutput" if debug else "Internal"

    def din(name, shape, dt=F32):
        return nc.dram_tensor(name, list(shape), dt, kind="ExternalInput").ap()

    x_own = din("x_own", [NT, D])
    x_prev = din("x_prev", [NT, D])
    c_col = din("c_col", [128, KC])
    pv_in = din("pv", [128, 1])
    w_ada = din("w_ada", [D, 6 * D])
    b_ada = din("b_ada", [1, 6 * D])
    w_ada_f = din("w_ada_f", [D, 2 * D])
    b_ada_f = din("b_ada_f", [1, 2 * D])
    g_mix_col = din("g_mix_col", [128, KC])
    w_in = din("w_in", [D, IN_COLS])
    conv_w_col = din("conv_w_col", [128, 8, 3])
    w_uk = din("w_uk", [8, 512, 128])
    w_uv = din("w_uv", [8, 512, 128])
    kv_g = din("kv_g", [1, 512])
    g_conv_col = din("g_conv_col", [128, 8])
    g_attn = din("g_attn", [1, 1024])
    w_out = din("w_out", [D, D])
    g_ffn_col = din("g_ffn_col", [128, KC])
    w_r = din("w_r", [D, 36])
    b_r = din("b_r", [1, 36])
    if upto >= 6:
        w_gate = din("w_gate", [NEXP, D, FF])
        w_up = din("w_up", [NEXP, D, FF])
        w_down = din("w_down", [NEXP, FF, D])
    g_final = din("g_final", [1, D])
    ident_bf = din("ident_bf", [128, 128], BF16)
    ident_f = din("ident_f", [128, 128])
    tri_in = din("tri", [128, 128])
    pow2_in = din("pow2", [128, NIT + 2])
    out = nc.dram_tensor("out", [NT, D], F32, kind="ExternalOutput").ap()

    modrow = nc.dram_tensor("modrow", [1, 8 * D], F32, kind=skind).ap()
    projT = nc.dram_tensor("projT", [48, 128, NT], BF16, kind=skind).ap()
    haloT = nc.dram_tensor("haloT", [128, 16, 2], BF16, kind=skind).ap()
    mixT_d = nc.dram_tensor("mixT", [16, 128, NT], BF16, kind=skind).ap()

    with ExitStack() as gs:
        def sb(name, shape, dt=F32, st=gs):
            return st.enter_context(nc.sbuf_tensor(name, list(shape), dt))

        def ps(name, shape, dt=F32, st=gs):
            return st.enter_context(nc.psum_tensor(name, list(shape), dt))

        pool = SemPool(nc, gs)
        idb = sb("idb", [128, 128], BF16)
        idf = sb("idf", [128, 128])
        ones_bf = sb("ones_bf", [128, 128], BF16)
        eps_t = sb("eps_t", [128, 1])
        pv = sb("pv_t", [128, 1])
        pvbias = sb("pvbias", [128, 1])
        cact = sb("cact", [128, KC], BF16)
        kst = ExitStack()
        ckv_tok = sb("ckv_tok", [128, 16, 512], BF16, st=kst)
        ckvT = sb("ckvT", [128, 4, SEQ], BF16, st=kst)
        ikT = sb("ikT", [128, SEQ], BF16, st=kst)
        iw_sb = sb("iw_sb", [128, 8, 16], st=kst)
        S = Sched(nc, pool)
        Bc = Buf("cc", persist=True)
        B_const = Buf("const", persist=True)
        B_ckv = Buf("ckv", persist=True)
        B_ik = Buf("ik", persist=True)
        B_iw = Buf("iw", persist=True)
        B_mod = Buf("modrow", persist=True)
        B_proj = Buf("projT", persist=True)
        B_halo = Buf("haloT", persist=True)
        B_mix = Buf("mixT", persist=True)

        def dma1(eng, o, i, reads=(), writes=(), **kw):
            return S.dma(eng, lambda e: [e.dma_start(out=o, in_=i, **kw)], 1, reads=reads, writes=writes)

        def dmaN(eng, pairs, reads=(), writes=(), **kw):
            return S.dma(eng, lambda e: [e.dma_start(out=o, in_=i, **kw) for (o, i) in pairs], len(pairs),
                         reads=reads, writes=writes)

        def mmg(out_ap, pairs, reads, writes):
            n = len(pairs)

            def fn(e):
                r = None
                for k, (l, rr) in enumerate(pairs):
                    r = e.matmul(out_ap, lhsT=l, rhs=rr, start=(k == 0), stop=(k == n - 1))
                return r
            return S.op("pe", fn, reads=reads, writes=writes)

        def mm_multi(groups, reads, writes):
            def fn(e):
                r = None
                for (o, pairs) in groups:
                    n = len(pairs)
                    for k, (l, rr) in enumerate(pairs):
                        r = e.matmul(o, lhsT=l, rhs=rr, start=(k == 0), stop=(k == n - 1))
                return r
            return S.op("pe", fn, reads=reads, writes=writes)

        def transposes(items, reads, writes):
            def fn(e):
                r = None
                for (o, i) in items:
                    r = e.transpose(o, i, idb[:, :])
                return r
            return S.op("pe", fn, reads=reads, writes=writes)

        def act(out_ap, in_ap, func, reads, writes, **kw):
            return S.op("act", lambda e: e.activation(out=out_ap, in_=in_ap, func=func, **kw),
                        reads=reads, writes=writes)

        def tsc(eng, out_ap, in_ap, s1, s2, op0, op1, reads, writes, **kw):
            if s2 is None and op1 is None:
                return S.op(eng, lambda e: e.tensor_scalar(out_ap, in_ap, s1, None, op0, **kw),
                            reads=reads, writes=writes)
            return S.op(eng, lambda e: e.tensor_scalar(out_ap, in_ap, s1, s2, op0, op1, **kw),
                        reads=reads, writes=writes)

        def tt(eng, out_ap, a, b, op, reads, writes):
            return S.op(eng, lambda e: e.tensor_tensor(out_ap, a, b, op), reads=reads, writes=writes)

        def stt(out_ap, in0, scalar, in1, op0, op1, reads, writes):
            return S.op("dve", lambda e: e.scalar_tensor_tensor(out_ap, in0, scalar, in1, op0, op1),
                        reads=reads, writes=writes)

        def cpy(eng, out_ap, in_ap, reads, writes):
            if eng == "act":
                return act(out_ap, in_ap, AF.Identity, reads, writes)
            return S.op(eng, lambda e: e.tensor_copy(out_ap, in_ap), reads=reads, writes=writes)

        def rstd_from_ssq(rs_ap, ssq_ap, inv_n, buf, reads=()):
            act(rs_ap, ssq_ap, AF.Sqrt, [buf, B_const] + list(reads), [buf], scale=inv_n, bias=eps_t[:, 0:1])
            S.op("dve", lambda e: e.reciprocal(rs_ap, rs_ap), reads=[buf], writes=[buf])

        dma1("sp", idb[:, :], ident_bf[:, :], writes=[B_const])
        dma1("sp", idf[:, :], ident_f[:, :], writes=[B_const])
        dma1("sp", pv[:, :], pv_in[:, :], writes=[B_const])
        S.op("dve", lambda e: e.memset(ones_bf[:, :], 1.0), writes=[B_const])
        S.op("dve", lambda e: e.memset(eps_t[:, :], EPS), writes=[B_const])
        tsc("dve", pvbias[:, :], pv[:, :], -1.0, BIG, ALU.add, ALU.mult, [B_const], [B_const])
        S.flush()

        with ExitStack() as ph:
            cc = sb("cc", [128, KC], st=ph)
            wbuf = [sb("wbuf%d" % i, [128, KC, 1024], BF16, st=ph) for i in range(2)]
            brow = [sb("brow%d" % i, [1, 1024], st=ph) for i in range(2)]
            mrow = [sb("mrow%d" % i, [1, 1024], st=ph) for i in range(2)]
            pm = [ps("pm%d" % i, [1, 512], st=ph) for i in range(2)]
            Bw = [Buf("wbuf0"), Buf("wbuf1")]
            Bb = [Buf("brow0"), Buf("brow1")]
            Bm = [Buf("mrow0"), Buf("mrow1")]
            Bp = [Buf("pm0"), Buf("pm1")]
            dma1("sp", cc[:, :], c_col[:, :], writes=[Bc])
            act(cact[:, :], cc[:, :], AF.Silu, [Bc], [Bc])
            for g in range(4):
                wsrc, bsrc, c0 = w_ada, b_ada, g * 1024
                wv = wsrc[:, c0:c0 + 1024].rearrange("(k p) n -> p k n", p=128)
                i2 = g % 2
                dmaN("pool", [(wbuf[i2][:, 4 * q:4 * q + 4, :], wv[:, 4 * q:4 * q + 4, :]) for q in range(4)],
                     writes=[Bw[i2]])
                dma1("sp", brow[i2][:, :], bsrc[0:1, c0:c0 + 1024], writes=[Bb[i2]])
                for j in range(2):
                    mmg(pm[j][:, :], [(cact[:, k:k + 1], wbuf[i2][:, k, j * 512:(j + 1) * 512]) for k in range(KC)],
                        [Bc, Bw[i2]], [Bp[j]])
                    tt("dve", mrow[i2][:, j * 512:(j + 1) * 512], pm[j][:, :], brow[i2][:, j * 512:(j + 1) * 512],
                       ALU.add, [Bp[j], Bb[i2]], [Bm[i2]])
                dma1("sp", modrow[0:1, g * 1024:(g + 1) * 1024], mrow[i2][:, :], reads=[Bm[i2]], writes=[B_mod])
            S.flush()
        if upto <= 0:
            return nc

        def load_mod_col(dst, off, buf, eng="sp"):
            src = modrow[0:1, off:off + D].rearrange("o (k p) -> p (o k)", p=128)
            return dma1(eng, dst, src, reads=[B_mod], writes=[buf], allow_slow_non_contiguous=True)

        def load_bc(dst, src_row, n, buf, reads=(), eng="sp"):
            return dma1(eng, dst, src_row.partition_broadcast(128).rearrange("p o n -> p (o n)"),
                        reads=list(reads), writes=[buf])

        with ExitStack() as ph:
            A1 = sb("A1", [128, KC], st=ph)
            B1 = sb("B1", [128, KC], st=ph)
            sc1 = sb("sc1", [128, KC], st=ph)
            gmc = sb("gmc", [128, KC], st=ph)
            gkv_bc = sb("gkv_bc", [128, 512], st=ph)
            hT = sb("hT", [128, KC, SEQ], BF16, st=ph)
            xt = [sb("xt%d" % i, [128, D], st=ph) for i in range(2)]
            xn = sb("xn", [128, 4, D], BF16, st=ph)
            junk = sb("junk", [128, D], BF16, st=ph)
            ssq = sb("ssq", [128, 16], st=ph)
            rs = sb("rs", [128, 16], st=ph)
            ssk = sb("ssk", [128, 16], st=ph)
            rsk = sb("rsk", [128, 16], st=ph)
            wkv = sb("wkv", [128, KC, 512], BF16, st=ph)
            wik = sb("wik", [128, KC, 128], BF16, st=ph)
            wiw = sb("wiw", [128, KC, 16], BF16, st=ph)
            wg = [sb("wg%d" % i, [128, KC, 512], BF16, st=ph) for i in range(2)]
            stg = [sb("stg%d" % i, [128, NT], BF16, st=ph) for i in range(2)]
            hstg = [sb("hstg%d" % i, [128, 2], BF16, st=ph) for i in range(2)]
            tp = [ps("tp%d" % i, [128, 512], BF16, st=ph) for i in range(2)]
            pkv = [ps("pkv%d" % i, [128, 512], st=ph) for i in range(2)]
            pik = ps("pik", [128, 512], st=ph)
            pp = [ps("pp%d" % i, [128, 512], st=ph) for i in range(2)]
            ph2 = ps("ph2", [128, 16], st=ph)
            BA = Buf("A1B1")
            Bx = [Buf("xt0"), Buf("xt1")]
            Bxn = [Buf("xn%d" % i) for i in range(4)]
            Bjunk = Buf("junk")
            Bss = Buf("ssq")
            BhT = [Buf("hT%d" % i) for i in range(4)]
            Btp = [Buf("tp0"), Buf("tp1")]
            Bwkv = Buf("wkv")
            Bpkv = [Buf("pkv0"), Buf("pkv1")]
            Bpik = Buf("pik")
            Bwg = [Buf("wg0"), Buf("wg1")]
            Bstg = [Buf("stg0"), Buf("stg1")]
            Bhstg = [Buf("hstg0"), Buf("hstg1")]
            Bpp = [Buf("pp0"), Buf("pp1")]
            Bph2 = Buf("ph2")
            Bssk = Buf("ssk")

            load_mod_col(sc1[:, :], 1 * D, BA, eng="pool")
            load_mod_col(B1[:, :], 0 * D, BA, eng="pool")
            dma1("pool", gmc[:, :], g_mix_col[:, :], writes=[BA])
            stt(A1[:, :], sc1[:, :], 1.0, gmc[:, :], ALU.add, ALU.mult, [BA], [BA])
            load_bc(gkv_bc[:, :], kv_g[0:1, :], 512, BA, eng="pool")
            wv = w_in.rearrange("(k p) n -> p k n", p=128)
            dmaN("pool", [(wkv[:, 8 * q:8 * q + 8, :], wv[:, 8 * q:8 * q + 8, OFF_KV:OFF_KV + 512]) for q in range(2)],
                 writes=[Bwkv])
            dma1("pool", wik[:, :, :], wv[:, :, OFF_IK:OFF_IK + 128], writes=[Bwkv])
            dma1("pool", wiw[:, :, :], wv[:, :, OFF_IW:OFF_IW + 16], writes=[Bwkv])

            def F1(g):
                for t4 in range(4):
                    F1tile(g, t4)

            def F1tile(g, t4):
                xsrc = x_prev if g < 2 else x_own
                if True:
                    t = g * 4 + t4
                    r0 = (t % 8) * 128
                    i2 = t % 2
                    dma1("sp", xt[i2][:, :], xsrc[r0:r0 + 128, :], writes=[Bx[i2]])
                    act(junk[:, :], xt[i2][:, :], AF.Square, [Bx[i2]], [Bjunk, Bss], accum_out=ssq[:, t:t + 1])
                    rstd_from_ssq(rs[:, t:t + 1], ssq[:, t:t + 1], 1.0 / D, Bss)
                    tsc("dve", xn[:, t4, :], xt[i2][:, :], rs[:, t:t + 1], None, ALU.mult, None,
                        [Bx[i2], Bss], [Bxn[t4]])

            def F2(g):
                for k in range(KC):
                    i2 = k % 2
                    transposes([(tp[i2][:, t4 * 128:(t4 + 1) * 128], xn[:, t4, k * 128:(k + 1) * 128]) for t4 in range(4)],
                               Bxn + [B_const], [Btp[i2]])
                    if k % 2 == 0:
                        act(hT[:, k, g * 512:(g + 1) * 512], tp[i2][:, :], AF.Identity, [Btp[i2], BA], [BhT[g]],
                            scale=A1[:, k:k + 1], bias=B1[:, k:k + 1])
                    else:
                        tsc("dve", hT[:, k, g * 512:(g + 1) * 512], tp[i2][:, :], A1[:, k:k + 1], B1[:, k:k + 1],
                            ALU.mult, ALU.add, [Btp[i2], BA], [BhT[g]])

            def Ktile(g, t4):
                if True:
                    t = g * 4 + t4
                    i2 = t % 2
                    mmg(pkv[i2][:, :], [(hT[:, k, t * 128:(t + 1) * 128], wkv[:, k, :]) for k in range(KC)],
                        [BhT[g], Bwkv], [Bpkv[i2]])
                    act(junk[:, 0:512], pkv[i2][:, :], AF.Square, [Bpkv[i2]], [Bjunk, Bssk], accum_out=ssk[:, t:t + 1])
                    rstd_from_ssq(rsk[:, t:t + 1], ssk[:, t:t + 1], 1.0 / 512, Bssk)
                    stt(ckv_tok[:, t, :], pkv[i2][:, :], rsk[:, t:t + 1], gkv_bc[:, :], ALU.mult, ALU.mult,
                        [Bpkv[i2], Bssk, BA], [B_ckv])
                    transposes([(tp[i2][:, rc * 128:(rc + 1) * 128], ckv_tok[:, t, rc * 128:(rc + 1) * 128]) for rc in range(4)],
                               [B_ckv, B_const], [Btp[i2]])
                    cpy("dve", ckvT[:, :, t * 128:(t + 1) * 128], tp[i2][:, :].rearrange("p (r q) -> p r q", r=4),
                        [Btp[i2]], [B_ckv])

            def Kik(g):
                mmg(pik[:, :], [(wik[:, k, :], hT[:, k, g * 512:(g + 1) * 512]) for k in range(KC)],
                    [BhT[g], Bwkv], [Bpik])
                cpy("dve", ikT[:, g * 512:(g + 1) * 512], pik[:, :], [Bpik], [B_ik])

            F1(0)
            F2(0)
            for g in range(4):
                for t4 in range(4):
                    Ktile(g, t4)
                    if g + 1 < 4:
                        F1tile(g + 1, t4)
                Kik(g)
                if g + 1 < 4:
                    F2(g + 1)
            for tt8 in range(8):
                c0 = NT + tt8 * 128
                mmg(ph2[:, :], [(hT[:, k, c0:c0 + 128], wiw[:, k, :]) for k in range(KC)],
                    [BhT[2 + tt8 // 4], Bwkv], [Bph2])
                act(iw_sb[:, tt8, :], ph2[:, :], AF.Identity, [Bph2], [B_iw], scale=IW_SCALE)
            for gi in range(12):
                col0 = 512 * gi if gi < 8 else OFF_IQ + 512 * (gi - 8)
                i2 = gi % 2
                dmaN("pool", [(wg[i2][:, 8 * q:8 * q + 8, :], wv[:, 8 * q:8 * q + 8, col0:col0 + 512]) for q in range(2)],
                     writes=[Bwg[i2]])
                for j in range(4):
                    ch = gi * 4 + j
                    s2 = ch % 2
                    for tg in range(2):
                        mmg(pp[tg][:, :], [(wg[i2][:, k, j * 128:(j + 1) * 128], hT[:, k, NT + tg * 512:NT + (tg + 1) * 512])
                                           for k in range(KC)], [Bwg[i2], BhT[2 + tg]], [Bpp[tg]])
                        cpy("act" if tg == 0 else "dve", stg[s2][:, tg * 512:(tg + 1) * 512], pp[tg][:, :],
                            [Bpp[tg]], [Bstg[s2]])
                    dma1("sp", projT[ch, :, :], stg[s2][:, :], reads=[Bstg[s2]], writes=[B_proj])
                    if 8 <= ch < 24:
                        mmg(ph2[:, 0:2], [(wg[i2][:, k, j * 128:(j + 1) * 128], hT[:, k, NT - 2:NT]) for k in range(KC)],
                            [Bwg[i2], BhT[1]], [Bph2])
                        cpy("dve", hstg[s2][:, :], ph2[:, 0:2], [Bph2], [Bhstg[s2]])
                        dma1("sp", haloT[:, ch - 8, :], hstg[s2][:, :], reads=[Bhstg[s2]], writes=[B_halo])
            S.flush()
        if upto <= 1:
            return nc

        with ExitStack() as ph:
            bT = sb("bT", [128, 8, NT], BF16, st=ph)
            cT = sb("cT", [128, 8, NT], BF16, st=ph)
            xT = sb("xT", [128, 8, NT], BF16, st=ph)
            chal = sb("chal", [128, 8, 2], BF16, st=ph)
            xhal = sb("xhal", [128, 8, 2], BF16, st=ph)
            u = sb("u", [128, 8, NT + 2], st=ph)
            cw = sb("cw", [128, 8, 3], st=ph)
            gcc = sb("gcc", [128, 8], st=ph)
            acc = sb("acc", [128, 8, NT], st=ph)
            ysq = sb("ysq", [128, 8, NT], BF16, st=ph)
            rbc = sb("rbc", [128, NT], st=ph)
            mixc = sb("mixc", [128, 8, NT], BF16, st=ph)
            pss = [ps("pss%d" % i, [128, 512], st=ph) for i in range(2)]
            Bh_, Bcw, Brbc = Buf("hal"), Buf("cw"), Buf("rbc")
            Bb_ = [Buf("bT%d" % j) for j in range(8)]
            Bc_ = [Buf("cT%d" % j) for j in range(8)]
            Bx_ = [Buf("xT%d" % j) for j in range(8)]
            Bu = [Buf("u%d" % j) for j in range(8)]
            Bacc = [Buf("acc%d" % j) for j in range(8)]
            Bysq = [Buf("ysq%d" % j) for j in range(8)]
            Bmixc = [Buf("mixc%d" % j) for j in range(8)]
            Buh = Buf("uhalo")
            Bpss = [Buf("pss0"), Buf("pss1")]
            dma1("sp", chal[:, :, :], haloT[:, 0:8, :], reads=[B_halo], writes=[Bh_])
            dma1("sp", xhal[:, :, :], haloT[:, 8:16, :], reads=[B_halo], writes=[Bh_])
            dma1("sp", cw[:, :, :], conv_w_col[:, :, :], writes=[Bcw])
            dma1("sp", gcc[:, :], g_conv_col[:, :], writes=[Bcw])
            for j in range(8):
                dma1("sp", cT[:, j, :], projT[8 + j, :, :], reads=[B_proj], writes=[Bc_[j]])
                dma1("sp", xT[:, j, :], projT[16 + j, :, :], reads=[B_proj], writes=[Bx_[j]])
                dma1("sp", bT[:, j, :], projT[j, :, :], reads=[B_proj], writes=[Bb_[j]])
            stt(u[:, :, 0:2], chal[:, :, :], pv[:, 0:1], xhal[:, :, :], ALU.mult, ALU.mult, [Bh_, B_const], [Buh])
            for j in range(8):
                tt("dve", u[:, j, 2:NT + 2], cT[:, j, :], xT[:, j, :], ALU.mult, [Bc_[j], Bx_[j]], [Bu[j]])
                tsc("dve", acc[:, j, :], u[:, j, 2:NT + 2], cw[:, j, 2:3], None, ALU.mult, None, [Bu[j], Bcw], [Bacc[j]])
                stt(acc[:, j, :], u[:, j, 1:NT + 1], cw[:, j, 1:2], acc[:, j, :], ALU.mult, ALU.add,
                    [Bu[j], Buh, Bcw, Bacc[j]], [Bacc[j]])
                stt(acc[:, j, :], u[:, j, 0:NT], cw[:, j, 0:1], acc[:, j, :], ALU.mult, ALU.add,
                    [Bu[j], Buh, Bcw, Bacc[j]], [Bacc[j]])
                tt("pool", acc[:, j, :], acc[:, j, :], bT[:, j, :], ALU.mult, [Bacc[j], Bb_[j]], [Bacc[j]])
                act(ysq[:, j, :], acc[:, j, :], AF.Square, [Bacc[j]], [Bysq[j]])
            for tg in range(2):
                mmg(pss[tg][:, :], [(ones_bf[:, :], ysq[:, j, tg * 512:(tg + 1) * 512]) for j in range(8)],
                    Bysq + [B_const], [Bpss[tg]])
                rstd_from_ssq(rbc[:, tg * 512:(tg + 1) * 512], pss[tg][:, :], 1.0 / 1024, Brbc, reads=[Bpss[tg]])
            for j in range(8):
                stt(mixc[:, j, :], acc[:, j, :], gcc[:, j:j + 1], rbc[:, :], ALU.mult, ALU.mult,
                    [Bacc[j], Bcw, Brbc], [Bmixc[j]])
                dma1("sp", mixT_d[j, :, :], mixc[:, j, :], reads=[Bmixc[j]], writes=[B_mix])
            S.flush()
        if upto <= 2:
            return nc

        with ExitStack() as ph:
            wuk_sb = sb("wuk_sb", [128, 8, 4, 128], BF16, st=ph)
            wuv_sb = sb("wuv_sb", [128, 8, 4, 128], BF16, st=ph)
            wukT = sb("wukT", [128, 8, 512], BF16, st=ph)
            gat_bc = sb("gat_bc", [128, 1024], st=ph)
            tri = sb("tri_t", [128, 128], st=ph)
            pow2 = sb("pow2_t", [128, NIT + 2], st=ph)
            qT = [sb("qT%d" % i, [128, 8, 128], BF16, st=ph) for i in range(3)]
            iqT = [sb("iqT%d" % i, [128, 16, 128], BF16, st=ph) for i in range(3)]
            qlat = sb("qlat", [128, 4, 8, 128], BF16, st=ph)
            score = sb("score", [128, SEQ], st=ph)
            cjunk = sb("cjunk", [128, SEQ], BF16, st=ph)
            rl = [sb("rl%d" % i, [128, 512], st=ph) for i in range(2)]
            mxmn = sb("mxmn", [128, 4], st=ph)
            Rs = sb("Rs", [128, NIT + 2], st=ph)
            mid = sb("mid", [128, 1], st=ph)
            cnt = sb("cnt", [128, 1], st=ph)
            tstep = sb("tstep", [128, 1], st=ph)
            lof = sb("lof", [128, 1], st=ph)
            selm = sb("selm", [128, SEQ], BF16, st=ph)
            selT = sb("selT", [128, 16, 128], BF16, st=ph)
            praw = [sb("praw%d" % i, [128, 512], BF16, st=ph) for i in range(2)]
            pT = [sb("pT%d" % i, [128, 16, 512], BF16, st=ph) for i in range(2)]
            olat = sb("olat", [128, 4, 8, 128], BF16, st=ph)
            lnd = sb("lnd", [128, 8], st=ph)
            rden = sb("rden", [128, 8], st=ph)
            ysb = sb("ysb", [128, 1024], st=ph)
            yjunk = sb("yjunk", [128, 1024], BF16, st=ph)
            yss = sb("yss", [128, 4], st=ph)
            yn = sb("yn", [128, 1024], BF16, st=ph)
            mixa = [sb("mixa%d" % i, [128, 8, 128], BF16, st=ph) for i in range(2)]
            pa = [ps("pa%d" % i, [128, 512], st=ph) for i in range(2)]
            pb = [ps("pb%d" % i, [128, 512], st=ph) for i in range(2)]
            ptb = [ps("ptb%d" % i, [128, 512], BF16, st=ph) for i in range(2)]
            pmisc = ps("pmisc", [128, 512], st=ph)
            pden = pmisc[:, 0:8]
            pm2 = pmisc[0:1, 0:MG]
            py = ps("py", [128, 512], st=ph)
            wb2 = [sb("wb2_%d" % i, [128, KC, MG], BF16, st=ph) for i in range(2)]
            brow2 = [sb("brow2_%d" % i, [1, MG], st=ph) for i in range(1)] * 2
            mrow2 = [sb("mrow2_%d" % i, [1, MG], st=ph) for i in range(1)] * 2
            Bwb2 = [Buf("wb2_%d" % i) for i in range(3)]
            Bbrow2 = [Buf("brow2_0")] * 2
            Bmrow2 = [Buf("mrow2_0")] * 2
            Bpy = Buf("py")
            Bw3 = Buf("w3")
            BqT = [Buf("qT%d" % i) for i in range(3)]
            BiqT = [Buf("iqT%d" % i) for i in range(3)]
            Bqlat, Bscore, Bcj, Bmm, Bbis, Bsel, BselT, Bolat, Brden, Bysb, Byn = [
                Buf(n) for n in "qlat score cjunk mxmn bis selm selT olat rden ysb yn".split()]
            BpT = [Buf("pT0"), Buf("pT1")]
            Brl = [Buf("rl0"), Buf("rl1")]
            Bpraw = [Buf("praw0"), Buf("praw1")]
            Bmixa = [Buf("mixa0"), Buf("mixa1")]
            Bpa = [Buf("pa0"), Buf("pa1")]
            Bpb = [Buf("pb0"), Buf("pb1")]
            Bptb = [Buf("ptb0"), Buf("ptb1")]
            Bpden = Buf("pden")
            Bpm2 = Bpden

            dma1("pool", wuk_sb[:, :, :, :], w_uk.rearrange("h (r p) d -> p h r d", p=128), writes=[Bw3])
            dma1("pool", wuv_sb[:, :, :, :], w_uv.rearrange("h (r p) d -> p h r d", p=128), writes=[Bw3])
            Bw3s = Buf("w3s")
            load_bc(gat_bc[:, :], g_attn[0:1, :], 1024, Bw3s)
            dma1("sp", tri[:, :], tri_in[:, :], writes=[Bw3s])
            dma1("sp", pow2[:, :], pow2_in[:, :], writes=[Bw3s])

            def LA(qi):
                q0 = qi * 128
                dma1("sp", iqT[qi % 3][:, :, :], projT[32:48, :, q0:q0 + 128].rearrange("h p t -> p h t"),
                     reads=[B_proj], writes=[BiqT[qi % 3]])

            def LB(qi):
                q0 = qi * 128
                dma1("sp", qT[qi % 3][:, :, :], projT[24:32, :, q0:q0 + 128].rearrange("h p t -> p h t"),
                     reads=[B_proj], writes=[BqT[qi % 3]])

            n_def = (16 * 1024 - 4096) // MG

            def DL(j):
                if j >= n_def:
                    return
                c = 4096 + j * MG
                wsrc, bsrc, c0 = (w_ada, b_ada, c) if c < 12 * 1024 else (w_ada_f, b_ada_f, c - 12 * 1024)
                wv_ = wsrc[:, c0:c0 + MG].rearrange("(k p) n -> p k n", p=128)
                b3 = j % 2
                dmaN("pool", [(wb2[b3][:, 8 * q:8 * q + 8, :], wv_[:, 8 * q:8 * q + 8, :]) for q in range(2)],
                     writes=[Bwb2[b3]])

            def DC(j):
                if j >= n_def:
                    return
                c = 4096 + j * MG
                b3 = j % 2
                bsrc, c0 = (b_ada, c) if c < 12 * 1024 else (b_ada_f, c - 12 * 1024)
                dma1("sp", brow2[b3][:, :], bsrc[0:1, c0:c0 + MG], writes=[Bbrow2[b3]])
                mmg(pm2, [(cact[:, k:k + 1], wb2[b3][:, k, :]) for k in range(KC)], [Bc, Bwb2[b3]], [Bpm2])
                act(mrow2[b3][:, :], pm2, AF.Identity, [Bpm2], [Bmrow2[b3]])
                tt("pool", mrow2[b3][:, :], mrow2[b3][:, :], brow2[b3][:, :], ALU.add, [Bmrow2[b3], Bbrow2[b3]], [Bmrow2[b3]])
                dma1("sp", modrow[0:1, c:c + MG], mrow2[b3][:, :], reads=[Bmrow2[b3]], writes=[B_mod])

            ndef = [0]
            DL(0)
            LA(0)
            LA(1)
            LB(0)
            for h in range(8):
                i2 = h % 2
                transposes([(ptb[i2][:, rc * 128:(rc + 1) * 128], wuk_sb[:, h, rc, :]) for rc in range(4)],
                           [Bw3, B_const], [Bptb[i2]])
                cpy("dve", wukT[:, h, :], ptb[i2][:, :], [Bptb[i2]], [Bw3])

            nmm = [0]

            def A1(qi):
                i3 = qi % 3
                n_s = 9 + qi
                nk = n_s * 128
                chunks = [(0, 512), (512, 512)]
                c0 = 1024
                while c0 < nk:
                    cwid = min(512, nk - c0)
                    chunks.append((c0, cwid))
                    c0 += cwid
                n_idx = 0
                for (c0, cwid) in chunks:
                    for h in range(16):
                        k2 = nmm[0] % 2
                        nmm[0] += 1
                        n_idx += 1
                        mmg(pb[k2][:, 0:cwid], [(iqT[i3][:, h, :], ikT[:, c0:c0 + cwid])], [BiqT[i3], B_ik], [Bpb[k2]])
                        act(rl[k2][:, 0:cwid], pb[k2][:, 0:cwid], AF.Relu, [Bpb[k2]], [Brl[k2]])
                        if h == 0:
                            tsc("dve", score[:, c0:c0 + cwid], rl[k2][:, 0:cwid], iw_sb[:, qi, 0:1], None,
                                ALU.mult, None, [Brl[k2], B_iw], [Bscore])
                        else:
                            stt(score[:, c0:c0 + cwid], rl[k2][:, 0:cwid], iw_sb[:, qi, h:h + 1], score[:, c0:c0 + cwid],
                                ALU.mult, ALU.add, [Brl[k2], B_iw, Bscore], [Bscore])
                S.op("dve", lambda e, nk=nk: e.tensor_reduce(mxmn[:, 0:1], score[:, 0:nk], AX.X, ALU.max),
                     reads=[Bscore], writes=[Bmm])
                S.op("dve", lambda e, nk=nk: e.tensor_reduce(mxmn[:, 1:2], score[:, 0:nk], AX.X, ALU.min),
                     reads=[Bscore], writes=[Bmm])
                tsc("dve", score[:, 0:1024], score[:, 0:1024], pvbias[:, 0:1], None, ALU.add, None,
                    [Bscore, B_const], [Bscore])
                tt("dve", score[:, nk - 128:nk], score[:, nk - 128:nk], tri[:, :], ALU.add, [Bscore, Bw3s], [Bscore])
                tsc("dve", mxmn[:, 2:3], mxmn[:, 0:1], mxmn[:, 1:2], None, ALU.subtract, None, [Bmm], [Bmm])
                tsc("dve", mxmn[:, 2:3], mxmn[:, 2:3], 1.002, 1e-9, ALU.mult, ALU.add, [Bmm], [Bmm])
                tsc("dve", Rs[:, :], pow2[:, :], mxmn[:, 2:3], None, ALU.mult, None, [Bmm, Bw3s], [Bbis])
                tsc("dve", mid[:, :], mxmn[:, 0:1], Rs[:, 0:1], None, ALU.subtract, None, [Bmm, Bbis], [Bbis])
                for it in range(NIT):
                    S.op("dve", lambda e, nk=nk: e.tensor_scalar(cjunk[:, 0:nk], score[:, 0:nk], mid[:, 0:1], None,
                                                                 ALU.is_gt, ALU.add, accum_out=cnt[:, 0:1]),
                         reads=[Bscore, Bbis], writes=[Bcj, Bbis])
                    tsc("dve", tstep[:, :], cnt[:, :], TOPK - 0.5, Rs[:, it:it + 1], ALU.is_ge, ALU.mult,
                        [Bbis], [Bbis])
                    tsc("dve", mid[:, :], mid[:, :], tstep[:, 0:1], Rs[:, it + 1:it + 2], ALU.add, ALU.subtract,
                        [Bbis], [Bbis])
                tsc("dve", lof[:, :], mid[:, :], Rs[:, NIT:NIT + 1], None, ALU.subtract, None, [Bbis], [Bbis])
                tsc("dve", selm[:, 0:nk], score[:, 0:nk], lof[:, 0:1], None, ALU.is_gt, None, [Bscore, Bbis], [Bsel])

            def A2(qi):
                n_s = 9 + qi
                for s4 in range(0, n_s, 4):
                    k2 = (s4 // 4) % 2
                    nn = min(4, n_s - s4)
                    transposes([(ptb[k2][:, a * 128:(a + 1) * 128], selm[:, (s4 + a) * 128:(s4 + a + 1) * 128])
                                for a in range(nn)], [Bsel, B_const], [Bptb[k2]])
                    cpy("act", selT[:, s4:s4 + nn, :], ptb[k2][:, 0:nn * 128].rearrange("p (a q) -> p a q", a=nn),
                        [Bptb[k2]], [BselT])

            def Bst(qi):
                i2 = qi % 2
                i3 = qi % 3
                n_s = 9 + qi
                q0 = qi * 128
                for rc in range(4):
                    for hg in range(2):
                        k2 = (rc * 2 + hg) % 2
                        mm_multi([(pa[k2][:, hh * 128:(hh + 1) * 128],
                                   [(wukT[:, hg * 4 + hh, rc * 128:(rc + 1) * 128], qT[i3][:, hg * 4 + hh, :])])
                                  for hh in range(4)], [Bw3, BqT[i3]], [Bpa[k2]])
                        cpy("act", qlat[:, rc, hg * 4:(hg + 1) * 4, :],
                            pa[k2][:, :].rearrange("p (h q) -> p h q", h=4), [Bpa[k2]], [Bqlat])
                n_it = 0
                for hg in range(2):
                    for st in range(n_s):
                        k2 = st % 2
                        n_it += 1
                        if n_it % 6 == 0 and n_it <= 18:
                            DL(ndef[0] + 1)
                            DC(ndef[0])
                            ndef[0] += 1
                        mmg(pa[k2][:, :], [(ckvT[:, rc, st * 128:(st + 1) * 128], qlat[:, rc, hg * 4:(hg + 1) * 4, :])
                                           for rc in range(4)], [B_ckv, Bqlat], [Bpa[k2]])
                        act(praw[k2][:, :], pa[k2][:, :], AF.Exp, [Bpa[k2]], [Bpraw[k2]], scale=ATT_SCALE)
                        tt("pool", pT[hg][:, st, :].rearrange("p (h q) -> p h q", h=4),
                           praw[k2][:, :].rearrange("p (h q) -> p h q", h=4),
                           selT[:, st, :].unsqueeze(1).to_broadcast([128, 4, 128]), ALU.mult,
                           [Bpraw[k2], BselT], [BpT[hg]])
                for hg in range(2):
                    for rc in range(4):
                        k2 = rc % 2
                        mmg(pb[k2][:, :], [(ckv_tok[:, st, rc * 128:(rc + 1) * 128], pT[hg][:, st, :]) for st in range(n_s)],
                            [B_ckv, BpT[hg]], [Bpb[k2]])
                        cpy("act", olat[:, rc, hg * 4:(hg + 1) * 4, :],
                            pb[k2][:, :].rearrange("p (h q) -> p h q", h=4), [Bpb[k2]], [Bolat])
                    mm_multi([(pden[:, hg * 4 + hh:hg * 4 + hh + 1],
                               [(pT[hg][:, st, hh * 128:(hh + 1) * 128], ones_bf[:, 0:1]) for st in range(n_s)])
                              for hh in range(4)], [BpT[hg], B_const], [Bpden])
                    mm_multi([(py[:, hh * 128:(hh + 1) * 128],
                               [(olat[:, rc, hg * 4 + hh, :], wuv_sb[:, hg * 4 + hh, rc, :]) for rc in range(4)])
                              for hh in range(4)], [Bolat, Bw3], [Bpy])
                    act(lnd[:, hg * 4:(hg + 1) * 4], pden[:, hg * 4:(hg + 1) * 4], AF.Ln, [Bpden], [Brden])
                    act(rden[:, hg * 4:(hg + 1) * 4], lnd[:, hg * 4:(hg + 1) * 4], AF.Exp, [Brden], [Brden], scale=-1.0)
                    for hh in range(4):
                        h = hg * 4 + hh
                        act(ysb[:, h * 128:(h + 1) * 128], py[:, hh * 128:(hh + 1) * 128], AF.Identity,
                            [Bpy, Brden], [Bysb], scale=rden[:, h:h + 1])
            def Bepi(qi):
                i2 = qi % 2
                q0 = qi * 128
                act(yjunk[:, :], ysb[:, :], AF.Square, [Bysb], [Byn], accum_out=yss[:, 0:1])
                act(yss[:, 1:2], yss[:, 0:1], AF.Ln, [Byn, B_const], [Byn], scale=1.0 / 1024, bias=eps_t[:, 0:1])
                act(yss[:, 2:3], yss[:, 1:2], AF.Exp, [Byn], [Byn], scale=-0.5)
                act(ysb[:, :], ysb[:, :], AF.Identity, [Bysb, Byn], [Bysb], scale=yss[:, 2:3])
                tt("pool", yn[:, :], ysb[:, :], gat_bc[:, :], ALU.mult, [Bysb, Bw3s], [Byn])
                for c4 in range(2):
                    transposes([(ptb[c4][:, a * 128:(a + 1) * 128], yn[:, (c4 * 4 + a) * 128:(c4 * 4 + a + 1) * 128])
                                for a in range(4)], [Byn, B_const], [Bptb[c4]])
                    cpy("act", mixa[i2][:, c4 * 4:(c4 + 1) * 4, :],
                        ptb[c4][:, :].rearrange("p (a q) -> p a q", a=4), [Bptb[c4]], [Bmixa[i2]])
                dma1("sp", mixT_d[8:16, :, q0:q0 + 128].rearrange("c p t -> p c t"), mixa[i2][:, :, :],
                     reads=[Bmixa[i2]], writes=[B_mix])

            A1(0)
            A2(0)
            for qi in range(8):
                if qi + 2 < 8:
                    LA(qi + 2)
                if qi + 1 < 8:
                    LB(qi + 1)
                    A1(qi + 1)
                if qi > 0:
                    Bepi(qi - 1)
                Bst(qi)
                if qi + 1 < 8:
                    A2(qi + 1)
            Bepi(7)
            while ndef[0] < n_def:
                DL(ndef[0] + 1)
                DC(ndef[0])
                ndef[0] += 1
            S.flush()
        if upto <= 3:
            return nc

        kst.close()
        xr = sb("xr", [128, 8, D])
        h2T = sb("h2T", [128, KC, NT], BF16)
        comb_tok = sb("comb_tok", [128, 8, NEXP])
        Bxr = [Buf("xr%d" % i, persist=True) for i in range(8)]
        Bh2 = Buf("h2T", persist=True)
        BcombT = Buf("comb_tok", persist=True)

        ph4 = ExitStack()
        if True:
            ph = ph4
            mixT = sb("mixT_s", [128, 16, NT], BF16, st=ph)
            gt1 = sb("gt1", [128, D], st=ph)
            wo = [sb("wo%d" % i, [128, KC, 512], BF16, st=ph) for i in range(2)]
            tmp = [sb("tmp%d" % i, [128, 512], st=ph) for i in range(2)]
            pw = [ps("pw%d" % i, [128, 512], st=ph) for i in range(2)]
            BmixS, Bgt1 = Buf("mixS"), Buf("gt1")
            Bwo = [Buf("wo0"), Buf("wo1")]
            Btmp = [Buf("tmp0"), Buf("tmp1")]
            Bpw = [Buf("pw0"), Buf("pw1")]
            dma1("sp", mixT[:, :, :], mixT_d.rearrange("c p t -> p c t"), reads=[B_mix], writes=[BmixS])
            load_bc(gt1[:, :], modrow[0:1, 2 * D:3 * D], D, Bgt1, reads=[B_mod])
            for t8 in range(8):
                dma1("sp", xr[:, t8, :], x_own[t8 * 128:(t8 + 1) * 128, :], reads=[BmixS], writes=[Bxr[t8]])
            wov = w_out.rearrange("(k p) n -> p k n", p=128)
            n = 0
            for ng in range(4):
                i2 = ng % 2
                dmaN("pool", [(wo[i2][:, 8 * q:8 * q + 8, :], wov[:, 8 * q:8 * q + 8, ng * 512:(ng + 1) * 512])
                              for q in range(2)], writes=[Bwo[i2]])
                for t8 in range(8):
                    k2 = n % 2
                    n += 1
                    mmg(pw[k2][:, :], [(mixT[:, k, t8 * 128:(t8 + 1) * 128], wo[i2][:, k, :]) for k in range(KC)],
                        [BmixS, Bwo[i2]], [Bpw[k2]])
                    tt("dve", tmp[k2][:, :], pw[k2][:, :], gt1[:, ng * 512:(ng + 1) * 512], ALU.mult,
                       [Bpw[k2], Bgt1], [Btmp[k2]])
                    tt("pool", xr[:, t8, ng * 512:(ng + 1) * 512], xr[:, t8, ng * 512:(ng + 1) * 512], tmp[k2][:, :],
                       ALU.add, [Btmp[k2], Bxr[t8]], [Bxr[t8]])
        if upto <= 4:
            S.flush()
            ph4.close()
            if debug:
                for t8 in range(8):
                    dma1("sp", out[t8 * 128:(t8 + 1) * 128, :], xr[:, t8, :], reads=[Bxr[t8]])
                S.flush()
            return nc

        with ExitStack() as ph:
            A2 = sb("A2", [128, KC], st=ph)
            B2 = sb("B2", [128, KC], st=ph)
            sc2 = sb("sc2", [128, KC], st=ph)
            gfc = sb("gfc", [128, KC], st=ph)
            xn = sb("xn5", [128, 4, D], BF16, st=ph)
            junk = sb("junk5", [128, D], BF16, st=ph)
            ssq = sb("ssq5", [128, 8], st=ph)
            rs = sb("rs5", [128, 8], st=ph)
            wr = sb("wr", [128, KC, 36], BF16, st=ph)
            br = sb("br", [128, 36], st=ph)
            lg = sb("lg", [128, 36], st=ph)
            sm = sb("sm", [128, 16], st=ph)
            oh = sb("oh", [128, 4], st=ph)
            ge = sb("ge", [128, 4], st=ph)
            em = sb("em", [128, 32], st=ph)
            pe_ = sb("pe_", [128, 32], st=ph)
            nm1 = sb("nm1", [128, 32], st=ph)
            pe2 = sb("pe2", [128, 32], st=ph)
            sel2 = sb("sel2", [128, 32], st=ph)
            tp = [ps("tp5%d" % i, [128, 512], BF16, st=ph) for i in range(2)]
            pr = ps("pr", [128, 36], st=ph)
            BA = Buf("A2B2")
            Bxn = [Buf("xn5%d" % i) for i in range(4)]
            Bj, Bss, Bwr, Blg, Bsm, Bcomb, Bpr, Bpct = [Buf(n) for n in "junk ss wr lg sm comb pr pct".split()]
            Btp = [Buf("tp0"), Buf("tp1")]
            load_mod_col(sc2[:, :], 4 * D, BA)
            load_mod_col(B2[:, :], 3 * D, BA)
            dma1("sp", gfc[:, :], g_ffn_col[:, :], writes=[BA])
            stt(A2[:, :], sc2[:, :], 1.0, gfc[:, :], ALU.add, ALU.mult, [BA], [BA])
            dma1("pool", wr[:, :, :], w_r.rearrange("(k p) n -> p k n", p=128), writes=[Bwr])
            Bbr = Buf("br")
            load_bc(br[:, :], b_r[0:1, :], 36, Bbr)
            def G1(g):
                for t4 in range(4):
                    t = g * 4 + t4
                    act(junk[:, :], xr[:, t, :], AF.Square, [Bxr[t]], [Bj, Bss], accum_out=ssq[:, t:t + 1])
                    rstd_from_ssq(rs[:, t:t + 1], ssq[:, t:t + 1], 1.0 / D, Bss)
                    tsc("dve", xn[:, t4, :], xr[:, t, :], rs[:, t:t + 1], None, ALU.mult, None, [Bxr[t], Bss], [Bxn[t4]])

            def G2(g):
                for k in range(KC):
                    i2 = k % 2
                    transposes([(tp[i2][:, t4 * 128:(t4 + 1) * 128], xn[:, t4, k * 128:(k + 1) * 128]) for t4 in range(4)],
                               Bxn + [B_const], [Btp[i2]])
                    if k % 2 == 0:
                        act(h2T[:, k, g * 512:(g + 1) * 512], tp[i2][:, :], AF.Identity, [Btp[i2], BA], [Bh2g[g]],
                            scale=A2[:, k:k + 1], bias=B2[:, k:k + 1])
                    else:
                        tsc("dve", h2T[:, k, g * 512:(g + 1) * 512], tp[i2][:, :], A2[:, k:k + 1], B2[:, k:k + 1],
                            ALU.mult, ALU.add, [Btp[i2], BA], [Bh2g[g]])

            Bh2g = [Buf("h2Tg0"), Buf("h2Tg1")]

            def router(t):
                    mmg(pr[:, :], [(h2T[:, k, t * 128:(t + 1) * 128], wr[:, k, :]) for k in range(KC)], [Bh2g[t // 4], Bwr], [Bpr])
                    tt("dve", lg[:, :], pr[:, :], br[:, :], ALU.add, [Bpr, Bbr], [Blg])
                    S.op("dve", lambda e: e.tensor_reduce(sm[:, 0:1], lg[:, 0:4], AX.X, ALU.max), reads=[Blg], writes=[Bsm])
                    tsc("dve", sm[:, 1:2], sm[:, 0:1], -1.0, None, ALU.mult, None, [Bsm], [Bsm])
                    act(ge[:, :], lg[:, 0:4], AF.Exp, [Blg, Bsm], [Bsm], bias=sm[:, 1:2], accum_out=sm[:, 2:3])
                    tsc("dve", oh[:, :], lg[:, 0:4], sm[:, 0:1], None, ALU.is_ge, None, [Blg, Bsm], [Bsm])
                    tsc("dve", oh[:, :], oh[:, :], -1.0, BIG, ALU.add, ALU.mult, [Bsm], [Bsm])
                    for g4 in range(4):
                        tsc("dve", em[:, g4 * 8:(g4 + 1) * 8], lg[:, 4 + g4 * 8:4 + (g4 + 1) * 8], oh[:, g4:g4 + 1], None,
                            ALU.add, None, [Blg, Bsm], [Bsm])
                    S.op("dve", lambda e: e.tensor_reduce(sm[:, 3:4], em[:, :], AX.X, ALU.max), reads=[Bsm], writes=[Bsm])
                    tsc("dve", sm[:, 4:5], sm[:, 3:4], -1.0, None, ALU.mult, None, [Bsm], [Bsm])
                    act(pe_[:, :], em[:, :], AF.Exp, [Bsm], [Bsm], bias=sm[:, 4:5])
                    S.op("dve", lambda e: e.tensor_reduce(sm[:, 5:6], pe_[:, :], AX.X, ALU.max), reads=[Bsm], writes=[Bsm])
                    tsc("dve", nm1[:, :], pe_[:, :], sm[:, 5:6], None, ALU.is_lt, None, [Bsm], [Bsm])
                    tt("dve", pe2[:, :], pe_[:, :], nm1[:, :], ALU.mult, [Bsm], [Bsm])
                    S.op("dve", lambda e: e.tensor_reduce(sm[:, 6:7], pe2[:, :], AX.X, ALU.max), reads=[Bsm], writes=[Bsm])
                    tsc("dve", sel2[:, :], pe_[:, :], sm[:, 6:7], None, ALU.is_ge, None, [Bsm], [Bsm])
                    tt("dve", sm[:, 7:8], sm[:, 5:6], sm[:, 6:7], ALU.add, [Bsm], [Bsm])
                    tt("dve", sm[:, 7:8], sm[:, 7:8], sm[:, 2:3], ALU.mult, [Bsm], [Bsm])
                    S.op("dve", lambda e: e.reciprocal(sm[:, 8:9], sm[:, 7:8]), reads=[Bsm], writes=[Bsm])
                    stt(comb_tok[:, t, :], pe_[:, :], sm[:, 8:9], sel2[:, :], ALU.mult, ALU.mult, [Bsm], [BcombT])
            G1(0)
            G2(0)
            G1(1)
            for t in range(4):
                router(t)
            G2(1)
            for t in range(4, 8):
                router(t)
            S.op("dve", lambda e: e.memset(sm[:, 15:16], 0.0), reads=[Bh2g[0], Bh2g[1]], writes=[Bh2])
            S.flush()
        ph4.close()
        if upto <= 5:
            return nc

        with ExitStack() as ph:
            gt2 = sb("gt2", [128, D], st=ph)
            wge = [sb("wge%d" % i, [128, KC, FF], BF16, st=ph) for i in range(2)]
            wue = [sb("wue%d" % i, [128, KC, FF], BF16, st=ph) for i in range(2)]
            wde = [sb("wde%d" % i, [128, 4, D], BF16, st=ph) for i in range(1)]
            sg = [sb("sg%d" % i, [128, 512], BF16, st=ph) for i in range(2)]
            hid = sb("hid", [128, 4, NT], BF16, st=ph)
            pA = [ps("pA%d" % i, [128, 512], st=ph) for i in range(2)]
            pU = [ps("pU%d" % i, [128, 512], st=ph) for i in range(2)]
            pD = [ps("pD%d" % i, [128, 512], st=ph) for i in range(4)]
            Bgt2 = Buf("gt2")
            Bwge = [Buf("wge0"), Buf("wge1")]
            Bwue = [Buf("wue0"), Buf("wue1")]
            Bwde = [Buf("wde_a"), Buf("wde_b")]
            Bsg = [Buf("sg0"), Buf("sg1")]
            Bhid = Buf("hid")
            BpA = [Buf("pA0"), Buf("pA1")]
            BpU = [Buf("pU0"), Buf("pU1")]
            BpD = [Buf("pD%d" % i) for i in range(4)]
            load_bc(gt2[:, :], modrow[0:1, 5 * D:6 * D], D, Bgt2, reads=[B_mod])

            def load_gu(e_):
                i2 = e_ % 2
                gv = w_gate[e_].rearrange("(k p) f -> p k f", p=128)
                uv = w_up[e_].rearrange("(k p) f -> p k f", p=128)
                dmaN("pool", [(wge[i2][:, 8 * q:8 * q + 8, :], gv[:, 8 * q:8 * q + 8, :]) for q in range(2)],
                     writes=[Bwge[i2]])
                dmaN("pool", [(wue[i2][:, 8 * q:8 * q + 8, :], uv[:, 8 * q:8 * q + 8, :]) for q in range(2)],
                     writes=[Bwue[i2]])

            def load_d_half(e_, hf):
                dv = w_down[e_].rearrange("(c p) d -> p c d", p=128)
                c0 = hf * 1024
                dmaN("pool", [(wde[0][:, 2 * q:2 * q + 2, c0:c0 + 1024], dv[:, 2 * q:2 * q + 2, c0:c0 + 1024])
                              for q in range(2)], writes=[Bwde[hf]])

            def prescale_half(hf):
                c0 = hf * 1024
                for fc in range(4):
                    tt("dve", wde[0][:, fc, c0:c0 + 1024], wde[0][:, fc, c0:c0 + 1024], gt2[:, c0:c0 + 1024], ALU.mult,
                       [Bwde[hf], Bgt2], [Bwde[hf]])

            load_gu(0)
            load_d_half(0, 0)
            load_d_half(0, 1)
            prescale_half(0)
            prescale_half(1)
            nd = 0
            for e_ in range(NEXP):
                i2 = e_ % 2
                if e_ + 1 < NEXP:
                    load_gu(e_ + 1)
                n = 0
                for tg in range(2):
                    for fc in range(4):
                        k2 = n % 2
                        n += 1
                        mmg(pA[k2][:, :], [(wge[i2][:, k, fc * 128:(fc + 1) * 128], h2T[:, k, tg * 512:(tg + 1) * 512])
                                           for k in range(KC)], [Bwge[i2], Bh2], [BpA[k2]])
                        mmg(pU[k2][:, :], [(wue[i2][:, k, fc * 128:(fc + 1) * 128], h2T[:, k, tg * 512:(tg + 1) * 512])
                                           for k in range(KC)], [Bwue[i2], Bh2], [BpU[k2]])
                        act(sg[k2][:, :], pA[k2][:, :], AF.Silu, [BpA[k2]], [Bsg[k2]])
                        tt("dve", hid[:, fc, tg * 512:(tg + 1) * 512], pU[k2][:, :], sg[k2][:, :], ALU.mult,
                           [BpU[k2], Bsg[k2]], [Bhid])
                        if e_ > 0 and n == 4:
                            prescale_half(0)
                        if e_ > 0 and n == 6:
                            prescale_half(1)
                for ng in range(4):
                    for t8 in range(8):
                        k2 = nd % 4
                        nd += 1
                        mmg(pD[k2][:, :], [(hid[:, fc, t8 * 128:(t8 + 1) * 128], wde[0][:, fc, ng * 512:(ng + 1) * 512])
                                           for fc in range(4)], [Bhid, Bwde[ng // 2]], [BpD[k2]])
                        stt(xr[:, t8, ng * 512:(ng + 1) * 512], pD[k2][:, :], comb_tok[:, t8, e_:e_ + 1],
                            xr[:, t8, ng * 512:(ng + 1) * 512], ALU.mult, ALU.add,
                            [BpD[k2], Bxr[t8], BcombT], [Bxr[t8]])
                    if ng % 2 == 1 and e_ + 1 < NEXP:
                        load_d_half(e_ + 1, ng // 2)
            S.flush()
        if upto <= 6:
            return nc

        with ExitStack() as ph:
            Af = sb("Af", [128, D], st=ph)
            gfb = sb("gfb", [128, D], st=ph)
            shf = sb("shf", [128, D], st=ph)
            junk = sb("junk7", [128, D], BF16, st=ph)
            ssq = sb("ssq7", [128, 8], st=ph)
            rs = sb("rs7", [128, 8], st=ph)
            ob = [sb("ob%d" % i, [128, D], st=ph) for i in range(2)]
            BAf, Bj, Bss = Buf("Af"), Buf("junk"), Buf("ss")
            Bob = [Buf("ob0"), Buf("ob1")]
            load_bc(Af[:, :], modrow[0:1, 7 * D:8 * D], D, BAf, reads=[B_mod])
            load_bc(shf[:, :], modrow[0:1, 6 * D:7 * D], D, BAf, reads=[B_mod])
            load_bc(gfb[:, :], g_final[0:1, :], D, BAf)
            stt(Af[:, :], Af[:, :], 1.0, gfb[:, :], ALU.add, ALU.mult, [BAf], [BAf])
            for t in range(8):
                i2 = t % 2
                act(junk[:, :], xr[:, t, :], AF.Square, [Bxr[t]], [Bj, Bss], accum_out=ssq[:, t:t + 1])
                rstd_from_ssq(rs[:, t:t + 1], ssq[:, t:t + 1], 1.0 / D, Bss)
                stt(ob[i2][:, :], xr[:, t, :], rs[:, t:t + 1], Af[:, :], ALU.mult, ALU.mult, [Bxr[t], Bss, BAf], [Bob[i2]])
                tt("pool", ob[i2][:, 0:1280], ob[i2][:, 0:1280], shf[:, 0:1280], ALU.add, [Bob[i2], BAf], [Bob[i2]])
                tt("dve", ob[i2][:, 1280:D], ob[i2][:, 1280:D], shf[:, 1280:D], ALU.add, [Bob[i2], BAf], [Bob[i2]])
                dma1("sp", out[t * 128:(t + 1) * 128, :], ob[i2][:, :], reads=[Bob[i2]])
            S.flush()
    return nc


def make_in_maps(x, c, w_ada, b_ada, g_mix, w_in, conv_w, w_uk, kv_norm_g, w_uv, g_conv_out, g_attn_out,
                 w_out, g_ffn, w_rg, b_rg, w_re, b_re, w_gate, w_up, w_down, w_ada_f, b_ada_f, g_final):
    f = np.float32
    a = lambda v: np.ascontiguousarray(np.asarray(v, dtype=f))
    col = lambda v: a(np.asarray(v).reshape(-1, 128).T)
    x = a(x)
    c = a(c)
    tri = np.where(np.arange(128)[None, :] <= np.arange(128)[:, None], 0.0, -BIG).astype(f)
    pow2 = np.broadcast_to((2.0 ** -(np.arange(NIT + 2) + 1.0)).astype(f)[None, :], (128, NIT + 2))
    shared = {
        "w_ada": a(w_ada[0]), "b_ada": a(b_ada[0]).reshape(1, -1),
        "w_ada_f": a(w_ada_f), "b_ada_f": a(b_ada_f).reshape(1, -1),
        "g_mix_col": col(g_mix[0]), "w_in": a(w_in[0]),
        "conv_w_col": a(np.asarray(conv_w[0]).reshape(3, 8, 128).transpose(2, 1, 0)),
        "w_uk": a(w_uk[0]), "w_uv": a(w_uv[0]), "kv_g": a(kv_norm_g[0]).reshape(1, -1),
        "g_conv_col": col(g_conv_out[0]), "g_attn": a(g_attn_out[0]).reshape(1, -1),
        "w_out": a(w_out[0]), "g_ffn_col": col(g_ffn[0]),
        "w_r": a(np.concatenate([np.asarray(w_rg[0]), np.asarray(w_re[0])], axis=1)),
        "b_r": a(np.concatenate([np.asarray(b_rg[0]), np.asarray(b_re[0])])).reshape(1, -1),
        "w_gate": a(w_gate[0]), "w_up": a(w_up[0]), "w_down": a(w_down[0]),
        "g_final": a(g_final).reshape(1, -1),
        "ident_bf": np.eye(128, dtype=ml_dtypes.bfloat16), "ident_f": np.eye(128, dtype=f),
        "tri": tri, "pow2": a(pow2),
    }
    maps = []
    for core in range(8):
        b, half = core // 2, core % 2
        m = dict(shared)
        m["x_own"] = a(x[b, half * NT:(half + 1) * NT])
        m["x_prev"] = a(x[b, 0:NT])
        m["c_col"] = col(c[b])
        m["pv"] = np.full((128, 1), float(half), dtype=f)
        maps.append(m)
    return maps


_NC_CACHE = {}


def kernel(**inputs):
    maps = make_in_maps(**inputs)
    if "nc" not in _NC_CACHE:
        _NC_CACHE["nc"] = build_nc()
    res = run_bass_kernel_spmd(_NC_CACHE["nc"], maps, core_ids=list(range(8)))
    outp = np.empty((4, SEQ, D), dtype=np.float32)
    for core in range(8):
        b, half = core // 2, core % 2
        outp[b, half * NT:(half + 1) * NT] = res.results[core]["out"]
    return outp
```
